# Optimizing a Trainium2 kernel written in Bass

```python
import math
import jax, jax.numpy as jnp
from jax import lax
import numpy as np

D_MODEL = 1024
BATCH = 8
SEQ = 4096
DEPTH = 4

CHUNK = 64
N_MEM = 256
EPS = 1e-6
NEG_BIG = -1e30
F_FLOOR = 1e-20
N_MIXERS = 2
HG_EXPAND = 128
HG_HEADS = D_MODEL // HG_EXPAND
HG_DK = HG_EXPAND
HG_DV = D_MODEL // HG_HEADS
HG_IN = 4 * D_MODEL
ML_HEADS = 8
ML_DV = D_MODEL // ML_HEADS
ML_DQK = ML_DV // 2
ML_IN = 2 * ML_HEADS * ML_DQK + 2 * D_MODEL + 2 * ML_HEADS
XA_HEADS = 4
XA_HD = D_MODEL // XA_HEADS
D_FF = 2816
CONV_W = 3
N_A = (DEPTH + 1) // 2
N_B = DEPTH // 2

kernel_name = "hybrid_hgrn2_mlstm_memxattn_convffn"


def rmsnorm(x, g):
    xf = x.astype(jnp.float32)
    y = xf * lax.rsqrt(jnp.mean(xf * xf, axis=-1, keepdims=True) + EPS)
    return (y * g.astype(jnp.float32)).astype(x.dtype)


def head_rmsnorm(o, g):
    B, S, H, d = o.shape
    o = o * lax.rsqrt(jnp.mean(o * o, axis=-1, keepdims=True) + EPS)
    return o.reshape(B, S, H * d) * g.astype(jnp.float32)


def to_chunks(t, n_heads):
    B, S, _ = t.shape
    return t.reshape(B, S // CHUNK, CHUNK, n_heads, -1).transpose(1, 0, 3, 2, 4)


def from_chunks(t):
    nC, B, H, L, d = t.shape
    return t.transpose(1, 0, 3, 2, 4).reshape(B, nC * L, H, d)


def hgrn2_mixer(a, w_in, w_out, norm_g, lb):
    B, S, _ = a.shape
    q, z, v, g = jnp.split(a @ w_in, 4, axis=-1)
    q = jax.nn.silu(q.astype(jnp.float32))
    zf = z.astype(jnp.float32)
    lbf = lb.astype(jnp.float32)
    f = lbf + (1.0 - lbf) * jax.nn.sigmoid(zf)
    log_f = jnp.log(jnp.maximum(f, F_FLOOR))
    k = (1.0 - lbf) * jax.nn.sigmoid(-zf)
    qc = to_chunks(q, HG_HEADS)
    kc = to_chunks(k, HG_HEADS)
    vc = to_chunks(v.astype(jnp.float32), HG_HEADS)
    fc = to_chunks(log_f, HG_HEADS)
    causal = jnp.tril(jnp.ones((CHUNK, CHUNK), dtype=bool))

    def step(S_prev, inp):
        qb, kb, vb, lfb = inp
        b = jnp.cumsum(lfb, axis=2)
        diff = b[:, :, :, None, :] - b[:, :, None, :, :]
        decay = jnp.exp(jnp.where(causal[None, None, :, :, None], diff, NEG_BIG))
        A = jnp.einsum('bhtk,bhtsk,bhsk->bhts', qb, decay, kb)
        o = (jnp.einsum('bhts,bhsv->bhtv', A, vb)
             + jnp.einsum('bhtk,bhkv->bhtv', qb * jnp.exp(b), S_prev))
        bL = b[:, :, -1:, :]
        S_new = (jnp.exp(bL[:, :, 0, :])[..., None] * S_prev
                 + jnp.einsum('bhsk,bhsv->bhkv', kb * jnp.exp(bL - b), vb))
        return S_new, o

    S0 = jnp.zeros((B, HG_HEADS, HG_DK, HG_DV), jnp.float32)
    _, oc = lax.scan(step, S0, (qc, kc, vc, fc))
    o = head_rmsnorm(from_chunks(oc), norm_g) * jax.nn.silu(g.astype(jnp.float32))
    return o.astype(a.dtype) @ w_out


def mlstm_mixer(a, w_in, b_gate, w_out, norm_g):
    B, S, _ = a.shape
    nqk = ML_HEADS * ML_DQK
    proj = a @ w_in
    q, k, v, o_pre, gates = jnp.split(
        proj, [nqk, 2 * nqk, 2 * nqk + D_MODEL, 2 * nqk + 2 * D_MODEL], axis=-1)
    gates = gates.astype(jnp.float32) + b_gate.astype(jnp.float32)
    log_i = gates[..., :ML_HEADS]
    log_f = jax.nn.log_sigmoid(gates[..., ML_HEADS:])
    qc = to_chunks(q.astype(jnp.float32), ML_HEADS)
    kc = to_chunks(k.astype(jnp.float32) * (ML_DQK ** -0.5), ML_HEADS)
    vc = to_chunks(v.astype(jnp.float32), ML_HEADS)
    ic = to_chunks(log_i, ML_HEADS)[..., 0]
    fc = to_chunks(log_f, ML_HEADS)[..., 0]
    causal = jnp.tril(jnp.ones((CHUNK, CHUNK), dtype=bool))

    def step(carry, inp):
        C, n, m = carry
        qb, kb, vb, lf, li = inp
        b = jnp.cumsum(lf, axis=-1)
        logD = jnp.where(causal, b[..., :, None] - b[..., None, :] + li[..., None, :],
                         NEG_BIG)
        inter = b + m[..., None]
        m_t = jnp.maximum(inter, jnp.max(logD, axis=-1))
        Dm = jnp.exp(logD - m_t[..., None])
        w_inter = jnp.exp(inter - m_t)
        s = jnp.einsum('bhtd,bhsd->bhts', qb, kb) * Dm
        num = (jnp.einsum('bhts,bhsv->bhtv', s, vb)
               + w_inter[..., None] * jnp.einsum('bhtd,bhdv->bhtv', qb, C))
        den = jnp.sum(s, axis=-1) + w_inter * jnp.einsum('bhtd,bhd->bht', qb, n)
        h = num / jnp.maximum(jnp.abs(den), jnp.exp(-m_t))[..., None]
        bL = b[..., -1]
        logw = bL[..., None] - b + li
        m_new = jnp.maximum(bL + m, jnp.max(logw, axis=-1))
        w = jnp.exp(logw - m_new[..., None])
        dec = jnp.exp(bL + m - m_new)
        C_new = dec[..., None, None] * C + jnp.einsum('bhs,bhsd,bhsv->bhdv', w, kb, vb)
        n_new = dec[..., None] * n + jnp.einsum('bhs,bhsd->bhd', w, kb)
        return (C_new, n_new, m_new), h

    carry0 = (jnp.zeros((B, ML_HEADS, ML_DQK, ML_DV), jnp.float32),
              jnp.zeros((B, ML_HEADS, ML_DQK), jnp.float32),
              jnp.zeros((B, ML_HEADS), jnp.float32))
    _, hc = lax.scan(step, carry0, (qc, kc, vc, fc, ic))
    o = head_rmsnorm(from_chunks(hc), norm_g) * jax.nn.sigmoid(o_pre.astype(jnp.float32))
    return o.astype(a.dtype) @ w_out


def memory_cross_attn(a, memn, wq, wkv, wo):
    B, S, _ = a.shape
    q = (a @ wq).reshape(B, S, XA_HEADS, XA_HD)
    k, v = jnp.split(memn @ wkv, 2, axis=-1)
    k = k.reshape(B, -1, XA_HEADS, XA_HD)
    v = v.reshape(B, -1, XA_HEADS, XA_HD)
    s = jnp.einsum('bqhd,bkhd->bhqk', q, k).astype(jnp.float32) * (XA_HD ** -0.5)
    p = jax.nn.softmax(s, axis=-1).astype(v.dtype)
    o = jnp.einsum('bhqk,bkhd->bqhd', p, v).reshape(B, S, D_MODEL)
    return o @ wo


def conv_ffn(a, w_up, conv_w, conv_b, w_down):
    u = a @ w_up
    u = lax.conv_general_dilated(
        u, conv_w[:, None, :].astype(u.dtype), window_strides=(1,),
        padding=[(CONV_W - 1, 0)], dimension_numbers=('NWC', 'WIO', 'NWC'),
        feature_group_count=2 * D_FF) + conv_b
    gate, val = jnp.split(u, 2, axis=-1)
    return (jax.nn.silu(gate) * val) @ w_down


def setup_inputs(seed: int = 0) -> dict:
    key = jax.random.key(seed)
    ks = jax.random.split(key, 24)
    D = D_MODEL
    nrm = lambda k, shape, fan_in: jax.random.normal(k, shape, jnp.float32) * fan_in ** -0.5
    gain = lambda k, shape: 1.0 + 0.02 * jax.random.normal(k, shape, jnp.float32)
    b_i = 0.1 * jax.random.normal(ks[14], (N_B, ML_HEADS), jnp.float32)
    b_f = 3.0 + 0.5 * jax.random.normal(ks[15], (N_B, ML_HEADS), jnp.float32)
    return {
        "x": jax.random.normal(ks[0], (BATCH, SEQ, D), jnp.float32),
        "mem": jax.random.normal(ks[1], (BATCH, N_MEM, D), jnp.float32),
        "norm_mix_g": gain(ks[2], (DEPTH, D)),
        "norm_xa_g": gain(ks[3], (DEPTH, D)),
        "norm_mem_g": gain(ks[4], (DEPTH, D)),
        "norm_ffn_g": gain(ks[5], (DEPTH, D)),
        "hg_w_in": nrm(ks[6], (N_A, D, HG_IN), D),
        "hg_w_out": nrm(ks[7], (N_A, D, D), D),
        "hg_norm_g": gain(ks[8], (N_A, D)),
        "hg_lb_logits": 0.5 * jax.random.normal(ks[9], (DEPTH, HG_HEADS * HG_DK), jnp.float32),
        "ml_w_in": nrm(ks[10], (N_B, D, ML_IN), D),
        "ml_b_gate": jnp.concatenate([b_i, b_f], axis=-1),
        "ml_w_out": nrm(ks[11], (N_B, D, D), D),
        "ml_norm_g": gain(ks[12], (N_B, D)),
        "xa_wq": nrm(ks[13], (DEPTH, D, D), D),
        "xa_wkv": nrm(ks[16], (DEPTH, D, 2 * D), D),
        "xa_wo": nrm(ks[17], (DEPTH, D, D), D),
        "ffn_w_up": nrm(ks[18], (DEPTH, D, 2 * D_FF), D),
        "ffn_conv_w": nrm(ks[19], (DEPTH, CONV_W, 2 * D_FF), CONV_W),
        "ffn_conv_b": 0.02 * jax.random.normal(ks[20], (DEPTH, 2 * D_FF), jnp.float32),
        "ffn_w_down": nrm(ks[21], (DEPTH, D_FF, D), D_FF),
        "final_g": gain(ks[22], (D,)),
    }


def reference(x, mem, norm_mix_g, norm_xa_g, norm_mem_g, norm_ffn_g,
              hg_w_in, hg_w_out, hg_norm_g, hg_lb_logits,
              ml_w_in, ml_b_gate, ml_w_out, ml_norm_g,
              xa_wq, xa_wkv, xa_wo,
              ffn_w_up, ffn_conv_w, ffn_conv_b, ffn_w_down, final_g):
    p = jax.nn.softmax(hg_lb_logits.astype(jnp.float32), axis=0)
    lower_bounds = jnp.cumsum(p, axis=0) - p[0]
    h = x
    for layer in range(DEPTH):
        j = layer // N_MIXERS
        a = rmsnorm(h, norm_mix_g[layer])
        if layer % N_MIXERS == 0:
            h = h + hgrn2_mixer(a, hg_w_in[j], hg_w_out[j], hg_norm_g[j],
                                lower_bounds[layer])
        else:
            h = h + mlstm_mixer(a, ml_w_in[j], ml_b_gate[j], ml_w_out[j], ml_norm_g[j])
        a = rmsnorm(h, norm_xa_g[layer])
        memn = rmsnorm(mem, norm_mem_g[layer])
        h = h + memory_cross_attn(a, memn, xa_wq[layer], xa_wkv[layer], xa_wo[layer])
        a = rmsnorm(h, norm_ffn_g[layer])
        h = h + conv_ffn(a, ffn_w_up[layer], ffn_conv_w[layer], ffn_conv_b[layer],
                         ffn_w_down[layer])
    return rmsnorm(h, final_g)
```

```python
import numpy as np
import ml_dtypes
import concourse.bass as bass
import concourse.mybir as mybir
from concourse.bass_utils import run_bass_kernel_spmd
from contextlib import ExitStack

F32 = mybir.dt.float32
BF16 = mybir.dt.bfloat16
AF = mybir.ActivationFunctionType
ALU = mybir.AluOpType
AX = mybir.AxisListType

ENGS = ("pe", "act", "dve", "pool", "sp")

D = 1024
SEQ = 4096
NMEM = 256
DEPTH = 4
DFF = 2816
NFF = DFF // 128
EPS = 1e-6
HG_IN = 4096
ML_IN = 3088
NV_L = 232


class Prog:
    def __init__(self, nc):
        self.nc = nc
        self.ops = []
        self.stack = ExitStack()
        self.nt = 0
        self.psum_keys = set()

    def sb(self, shape, dtype, name=None):
        self.nt += 1
        return self.stack.enter_context(self.nc.sbuf_tensor("sb_" + (name or f"t{self.nt}"), list(shape), dtype))

    def ps(self, shape, dtype, name=None):
        self.nt += 1
        return self.stack.enter_context(self.nc.psum_tensor("ps_" + (name or f"p{self.nt}"), list(shape), dtype))

    def add(self, eng, fn, reads=(), writes=(), lane=None):
        self.ops.append((eng, fn, tuple(reads), tuple(writes), lane))

    def dma(self, q, out, in_, reads=(), writes=(), lane=None):
        assert lane is not None
        self.ops.append((q, (lambda e, o=out, i=in_: e.dma_start(out=o, in_=i)), tuple(reads), tuple(writes), lane))

    def finish(self, final_lanes=()):
        nc = self.nc
        ops = self.ops
        n = len(ops)
        last_w = {}
        readers = {}
        know = {e: {} for e in ENGS}
        vc = [None] * n
        waits = [None] * n
        signaled = [False] * n
        lane_cnt = {}
        dma_cnt = [0] * n
        for i, (eng, fn, reads, writes, lane) in enumerate(ops):
            deps = set()
            raw = set()
            for r in reads:
                j = last_w.get(r)
                if j is not None:
                    deps.add(j)
                    raw.add(j)
                if r in self.psum_keys:
                    for j in readers.get(r, ()):
                        deps.add(j)
            for w in writes:
                j = last_w.get(w)
                if j is not None:
                    deps.add(j)
                for j in readers.get(w, ()):
                    deps.add(j)
            kn = know[eng]
            wl = []
            for j in sorted(deps):
                je, _, _, _, jl = ops[j]
                if jl is not None:
                    key = ("L", jl)
                    val = dma_cnt[j]
                else:
                    if je == eng and lane is None and (eng == "pe" or j not in raw):
                        continue
                    key = je
                    val = j
                if kn.get(key, -1) >= val:
                    continue
                wl.append(j)
                if jl is None:
                    signaled[j] = True
                for k2, v2 in vc[j].items():
                    if kn.get(k2, -1) < v2:
                        kn[k2] = v2
            waits[i] = wl
            v = dict(kn)
            if lane is not None:
                c = lane_cnt.get(lane, 0) + 1
                lane_cnt[lane] = c
                dma_cnt[i] = c
                v[("L", lane)] = c
            else:
                v[eng] = i
            vc[i] = v
            for r in reads:
                readers.setdefault(r, []).append(i)
            for w in writes:
                last_w[w] = i
                readers[w] = []
        sig = [0] * n
        cnt = {e: 0 for e in ENGS}
        for i, (eng, fn, reads, writes, lane) in enumerate(ops):
            if lane is None and signaled[i]:
                cnt[eng] += 1
                sig[i] = cnt[eng]
        self.stats = dict(n_ops=n, signals=dict(cnt), lanes=len(lane_cnt),
                          n_waits=sum(len(w) for w in waits),
                          per_eng={e: sum(1 for o in ops if o[0] == e) for e in ENGS})
        st = self.stack
        esem = {e: st.enter_context(nc.semaphore(f"s_{e}")) for e in ENGS}
        lsem = {l: st.enter_context(nc.semaphore(f"l_{k}")) for k, l in enumerate(lane_cnt)}
        per_eng = {e: [] for e in ENGS}
        for i, op in enumerate(ops):
            per_eng[op[0]].append(i)

        def emit(eng_name, e):
            for i in per_eng[eng_name]:
                _, fn, _, _, lane = ops[i]
                for j in waits[i]:
                    je, _, _, _, jl = ops[j]
                    if jl is not None:
                        e.wait_ge(lsem[jl], 16 * dma_cnt[j])
                    else:
                        e.wait_ge(esem[je], sig[j])
                ins = fn(e)
                if lane is not None:
                    ins.then_inc(lsem[lane], 16)
                elif signaled[i]:
                    ins.then_inc(esem[eng_name], 1)
            if eng_name == "sp":
                for l in final_lanes:
                    e.wait_ge(lsem[l], 16 * lane_cnt[l])

        with nc.Block() as block:
            @block.tensor
            def _(e):
                emit("pe", e)

            @block.scalar
            def _(e):
                emit("act", e)

            @block.vector
            def _(e):
                emit("dve", e)

            @block.gpsimd
            def _(e):
                emit("pool", e)

            @block.sync
            def _(e):
                emit("sp", e)
        st.close()


class Ring:
    def __init__(self, P, name, n, shape, dtype, psum=False):
        self.name = name
        self.n = n
        self.i = 0
        self.tiles = [(P.ps if psum else P.sb)(shape, dtype, f"{name}{k}") for k in range(n)]
        if psum:
            for k in range(n):
                P.psum_keys.add((name, k))

    def next(self):
        k = self.i % self.n
        self.i += 1
        return self.tiles[k], (self.name, k)


def build(S=SEQ, T=1024, layers=(0, 1, 2, 3), final=True):
    nc = bass.Bass("TRN2", target_bir_lowering=False)
    P = Prog(nc)
    NW = S // T
    NS = T // 512
    NCH = T // 64
    L0 = layers[0]

    def din(name, shape, dt=F32):
        return nc.dram_tensor(name, list(shape), dt, kind="ExternalInput").ap()

    x_d = din("x", [S, D])
    mem_d = din("mem", [NMEM, D])
    vec_d = din("vecs", [128, DEPTH * NV_L + 16])
    cst_d = din("cst", [128, 128 + 64 + 64 + 512 + 1024])
    cbf_d = din("cbf", [128, 256], BF16)
    hgin_d = din("hg_w_in", [2, D, HG_IN])
    hgout_d = din("hg_w_out", [2, D, D])
    mlin_d = din("ml_w_in", [2, D, ML_IN])
    mlout_d = din("ml_w_out", [2, D, D])
    wq_d = din("xa_wq", [DEPTH, D, D])
    wkv_d = din("xa_wkv", [DEPTH, D, 2 * D])
    wo_d = din("xa_wo", [DEPTH, D, D])
    wup_d = din("ffn_w_up", [DEPTH, D, 2 * DFF])
    wdn_d = din("ffn_w_down", [DEPTH, DFF, D])
    out_d = nc.dram_tensor("out", [S, D], F32, kind="ExternalOutput").ap()

    hT = P.sb([128, 8, T], F32, "hT")
    aT = P.sb([128, 8, T + 2], BF16, "aT")
    oT = P.sb([128, 8, T], BF16, "oT")
    prod = P.sb([128, NFF, T], BF16, "prod")
    vecs = P.sb([128, DEPTH * NV_L + 16], F32, "vecs")
    cst = P.sb([128, 128 + 64 + 64 + 512 + 1024], F32, "cst")
    cbf = P.sb([128, 256], BF16, "cbf")
    lbt = P.sb([128, DEPTH, 8], F32, "lbt")
    omlt = P.sb([128, DEPTH, 8], F32, "omlt")
    memhT = P.sb([128, 8, NMEM], F32, "memhT")
    memnT = P.sb([128, 8, NMEM], BF16, "memnT")
    KT = P.sb([128, 8, NMEM], BF16, "KT")
    Vt = P.sb([128, 2, D], BF16, "Vt")
    ffhalo = P.sb([128, DEPTH, 8, 2], BF16, "ffhalo")
    Sst = P.sb([128, 2, 8, 128], F32, "Sst")
    Sbf = P.sb([128, 128], BF16, "Sbf")
    Cst = P.sb([64, 2, 8, 129], F32, "Cst")
    Cbf = P.sb([64, 128], BF16, "Cbf")
    nBf = P.sb([64, 128], BF16, "nBf")
    mlcar = P.sb([8, 2, 2], F32, "mlcar")

    ident = cst[:, 0:128]
    maskT = cst[0:64, 128:192]
    negmT = cst[0:64, 192:256]
    rmask = cst[:, 256:768]
    sel = cst[0:8, 768:1792]
    identb = cbf[:, 0:128]
    onesb = cbf[:, 128:256]

    def vcol(l, off, n=1):
        return vecs[:, l * NV_L + off: l * NV_L + off + n]
    O_GMIX, O_GXA, O_GMEM, O_GFFN, O_CW0, O_CW1, O_CW2, O_CB, O_MNG, O_LB, O_BI, O_BF = 0, 8, 16, 24, 32, 76, 120, 164, 208, 216, 224, 225

    wr = Ring(P, "w", 3, [128, 8, 512], BF16)
    wdr = Ring(P, "wd", 2, [128, NFF, 128], BF16)
    xin = Ring(P, "xin", 2, [128, D], F32)
    pmm = Ring(P, "pmm", 4, [128, 512], F32, psum=True)
    psm = Ring(P, "psm", 3, [128, 512], F32, psum=True)
    ptr = P.ps([128, 1024], BF16, "ptr")
    P.psum_keys.add("ptr")
    f512 = Ring(P, "f512", 9, [128, 512], F32)
    b512 = Ring(P, "b512", 6, [128, 512], BF16)
    sm = Ring(P, "sm", 8, [128, 4], F32)
    vtok = Ring(P, "vtok", 2, [64, 8, 130], BF16)
    ktok = Ring(P, "ktok", 2, [64, 8, 128], BF16)
    qx = Ring(P, "qx", 4, [128, 512], BF16)
    s64 = Ring(P, "s64", 4, [64, 64], BF16)
    d64 = Ring(P, "d64", 4, [64, 64], F32)
    _g8 = {k: P.sb([8, T + 1], F32, f"g8{k}") for k in ("li", "lf", "B", "Mg", "wi")}
    g8 = dict(li=_g8["li"], u=_g8["li"], lf=_g8["lf"], B=_g8["B"], m=_g8["B"], cl=_g8["B"], Mg=_g8["Mg"], wi=_g8["wi"])
    uT = P.sb([64, NCH, 8], F32, "uT")

    cnt = [0]

    def uid():
        cnt[0] += 1
        return cnt[0]

    def mm(out, lhsT, rhs, start, stop, reads, writes):
        P.add("pe", lambda e: e.matmul(out, lhsT=lhsT, rhs=rhs, start=start, stop=stop), reads, writes)

    def tr(out, in_, idn, reads, writes):
        P.add("pe", lambda e: e.transpose(out, in_, idn), reads, writes)

    def act(out, in_, func, reads, writes, scale=1.0, bias=None, accum=None, eng="act"):
        kw = {}
        if bias is not None:
            kw["bias"] = bias
        if accum is not None:
            kw["accum_out"] = accum
        P.add(eng, lambda e: e.activation(out=out, in_=in_, func=func, scale=scale, **kw), reads, writes)

    def tt(eng, out, in0, in1, op, reads, writes):
        P.add(eng, lambda e: e.tensor_tensor(out=out, in0=in0, in1=in1, op=op), reads, writes)

    def ts(eng, out, in0, s1, s2, op0, op1, reads, writes):
        P.add(eng, lambda e: e.tensor_scalar(out=out, in0=in0, scalar1=s1, scalar2=s2, op0=op0, op1=op1), reads, writes)

    def stt(out, in0, scalar, in1, op0, op1, reads, writes):
        P.add("dve", lambda e: e.scalar_tensor_tensor(out=out, in0=in0, scalar=scalar, in1=in1, op0=op0, op1=op1), reads, writes)

    def cp(eng, out, in_, reads, writes):
        if eng == "act":
            act(out, in_, AF.Copy, reads, writes)
        else:
            P.add(eng, lambda e: e.tensor_copy(out=out, in_=in_), reads, writes)

    def recip(out, in_, reads, writes):
        P.add("dve", lambda e: e.reciprocal(out=out, in_=in_), reads, writes)

    def wload(ring, parts):
        t, k = ring.next()
        for dst_fn, src in parts:
            P.dma("pool", dst_fn(t), src, writes=[k], lane=k)
        return t, k

    def rows(w2d):
        return w2d.rearrange("(c p) n -> p c n", p=128)

    P.dma("sp", vecs[:], vec_d, writes=["vecs"], lane="c0")
    P.dma("sp", cst[:], cst_d, writes=["cst"], lane="c1")
    P.dma("sp", cbf[:], cbf_d, writes=["cbf"], lane="c2")
    CR = ["vecs", "cst", "cbf"]

    ex = P.sb([128, DEPTH, 8], F32, "ex")
    mxl = P.sb([128, 8], F32, "mxl")
    sml = P.sb([128, 8], F32, "sml")
    lg = [vcol(l, O_LB, 8) for l in range(DEPTH)]
    tt("dve", mxl[:], lg[0], lg[1], ALU.max, CR, ["mxl"])
    tt("dve", mxl[:], mxl[:], lg[2], ALU.max, CR + ["mxl"], ["mxl"])
    tt("dve", mxl[:], mxl[:], lg[3], ALU.max, CR + ["mxl"], ["mxl"])
    for l in range(DEPTH):
        tt("dve", ex[:, l, :], lg[l], mxl[:], ALU.subtract, CR + ["mxl"], [("ex", l)])
        act(ex[:, l, :], ex[:, l, :], AF.Exp, [("ex", l)], [("ex", l)])
    tt("dve", sml[:], ex[:, 0, :], ex[:, 1, :], ALU.add, [("ex", 0), ("ex", 1)], ["sml"])
    tt("dve", sml[:], sml[:], ex[:, 2, :], ALU.add, ["sml", ("ex", 2)], ["sml"])
    tt("dve", sml[:], sml[:], ex[:, 3, :], ALU.add, ["sml", ("ex", 3)], ["sml"])
    recip(sml[:], sml[:], ["sml"], ["sml"])
    P.add("dve", lambda e: e.memset(lbt[:, 0, :], 0.0), [], [("lbt", 0)])
    for l in range(1, DEPTH):
        if l == 1:
            cp("dve", lbt[:, 1, :], ex[:, 1, :], [("ex", 1)], [("lbt", 1)])
        else:
            tt("dve", lbt[:, l, :], lbt[:, l - 1, :], ex[:, l, :], ALU.add, [("lbt", l - 1), ("ex", l)], [("lbt", l)])
    for l in range(1, DEPTH):
        pass
    for l in range(DEPTH - 1, 0, -1):
        tt("dve", lbt[:, l, :], lbt[:, l, :], sml[:], ALU.mult, [("lbt", l), "sml"], [("lbt", l)])
    for l in range(DEPTH):
        ts("dve", omlt[:, l, :], lbt[:, l, :], -1.0, 1.0, ALU.mult, ALU.add, [("lbt", l)], [("omlt", l)])

    P.add("pool", lambda e: e.memset(Sst[:], 0.0), [], ["Sst"])
    P.add("pool", lambda e: e.memset(Cst[:], 0.0), [], ["Cst"])
    P.add("pool", lambda e: e.memset(mlcar[:], 0.0), [], ["mlcar"])
    P.add("pool", lambda e: e.memset(ffhalo[:], 0.0), [], ["ffhalo"])
    for k_, r in enumerate(vtok.tiles):
        P.add("pool", lambda e, r=r: e.memset(r[:, :, 128:130], 1.0), [], [("vtok", k_)])

    for kt in range(2):
        xt, xk = xin.next()
        P.dma("sp", xt[:], mem_d[kt * 128:(kt + 1) * 128, :], writes=[xk], lane=xk)
        sq_t, sq_k = f512.next()
        st_, sk = sm.next()
        for hh in range(2):
            act(sq_t[:], xt[:, hh * 512:(hh + 1) * 512], AF.Square, [xk], [sq_k, sk], accum=st_[:, hh:hh + 1])
        tt("dve", st_[:, 2:3], st_[:, 0:1], st_[:, 1:2], ALU.add, [sq_k, sk], [sk])
        act(st_[:, 2:3], st_[:, 2:3], AF.Sqrt, [sk], [sk], scale=1.0 / D, bias=vecs[:, DEPTH * NV_L + 8: DEPTH * NV_L + 9])
        recip(st_[:, 3:4], st_[:, 2:3], [sk], [sk])
        ts("dve", xt[:], xt[:], st_[:, 3:4], None, ALU.mult, ALU.bypass, [xk, sk], [xk])
        for g in range(2):
            pt, pk = pmm.next()
            for c4 in range(4):
                c = g * 4 + c4
                tr(pt[:, c4 * 128:(c4 + 1) * 128], xt[:, c * 128:(c + 1) * 128], ident, [xk, "cst"], [pk])
            cp("act", memhT[:, g * 4:(g + 1) * 4, kt * 128:(kt + 1) * 128],
               pt[:].rearrange("p (c t) -> p c t", c=4), [pk], ["memhT"])

    EPSC = vecs[:, DEPTH * NV_L + 8: DEPTH * NV_L + 9]

    def rmsnorm_to_aT(gcol_off, l, halo=False):
        for s in range(NS):
            sl = slice(s * 512, (s + 1) * 512)
            pt, pk = pmm.next()
            for c in range(8):
                bt, bk = b512.next()
                act(bt[:], hT[:, c, sl], AF.Square, [("hT", c, s)], [bk], eng="act")
                mm(pt[:], onesb, bt[:], c == 0, c == 7, [bk, "cbf"], [pk])
            rt, rk = f512.next()
            act(rt[:], pt[:], AF.Sqrt, [pk, "vecs"], [rk], scale=1.0 / D, bias=EPSC)
            recip(rt[:], rt[:], [rk], [rk])
            for c in range(8):
                stt(aT[:, c, 2 + s * 512: 2 + (s + 1) * 512], hT[:, c, sl], vcol(l, gcol_off + c), rt[:],
                    ALU.mult, ALU.mult, [("hT", c, s), rk, "vecs"], [("aT", c, s)])

    def aTs(c, s):
        return aT[:, c, 2 + s * 512: 2 + (s + 1) * 512]

    def aT_reads(s):
        return [("aT", c, s) for c in range(8)]

    def out_proj(w_d2, tag):
        tiles = [wload(wr, [(lambda t: t[:], rows(w_d2)[:, :, hf * 512:(hf + 1) * 512])]) for hf in range(2)]
        for hf in range(2):
            wt, wk = tiles[hf]
            for m4 in range(4):
                m = hf * 4 + m4
                for s in range(NS):
                    pt, pk = pmm.next()
                    for c in range(8):
                        mm(pt[:], wt[:, c, m4 * 128:(m4 + 1) * 128], oT[:, c, s * 512:(s + 1) * 512], c == 0, c == 7,
                           [wk, ("oT", c, s)], [pk])
                    tt("dve", hT[:, m, s * 512:(s + 1) * 512], hT[:, m, s * 512:(s + 1) * 512], pt[:], ALU.add,
                       [pk, ("hT", m, s)], [("hT", m, s)])

    def head_norm_gate(src_ps_or_sb, src_key, gate_t, gate_k, l, h, s):
        hh_t, hh_k = f512.next()
        cp("act", hh_t[:], src_ps_or_sb, [src_key], [hh_k])
        bt, bk = b512.next()
        act(bt[:], hh_t[:], AF.Square, [hh_k], [bk])
        pt, pk = pmm.next()
        mm(pt[:], onesb, bt[:], True, True, [bk, "cbf"], [pk])
        rt, rk = f512.next()
        act(rt[:], pt[:], AF.Sqrt, [pk, "vecs"], [rk], scale=1.0 / 128, bias=EPSC)
        recip(rt[:], rt[:], [rk], [rk])
        stt(hh_t[:], hh_t[:], vcol(l, O_MNG + h), rt[:], ALU.mult, ALU.mult, [hh_k, rk, "vecs"], [hh_k])
        tt("dve", oT[:, h, s * 512:(s + 1) * 512], hh_t[:], gate_t[:], ALU.mult, [hh_k, gate_k], [("oT", h, s)])

    def hgrn2(l):
        j = l // 2
        w_in = hgin_d[j]
        rmsnorm_to_aT(O_GMIX, l)

        def load_head(h):
            return wload(wr, [(lambda t, q=q: t[:, :, q * 128:(q + 1) * 128],
                               rows(w_in)[:, :, q * 1024 + h * 128: q * 1024 + (h + 1) * 128]) for q in range(4)])
        nxt = load_head(0)
        for h in range(8):
            wt, wk = nxt
            if h + 1 < 8:
                nxt = load_head(h + 1)
            S_h = Sst[:, j, h, :]
            SK = ("Sst", j, h)
            for s in range(NS):
                ar = aT_reads(s)
                pq, pqk = pmm.next()
                pz, pzk = pmm.next()
                pg, pgk = pmm.next()
                for (pt, pk, q) in ((pq, pqk, 0), (pz, pzk, 1), (pg, pgk, 3)):
                    for c in range(8):
                        mm(pt[:], wt[:, c, q * 128:(q + 1) * 128], aTs(c, s), c == 0, c == 7, [wk] + ar, [pk])
                qs, qsk = f512.next()
                act(qs[:], pq[:], AF.Silu, [pqk], [qsk])
                gs, gsk = f512.next()
                act(gs[:], pg[:], AF.Silu, [pgk], [gsk])
                sg, sgk = f512.next()
                act(sg[:], pz[:], AF.Sigmoid, [pzk], [sgk])
                kk, kkk = f512.next()
                act(kk[:], pz[:], AF.Sigmoid, [pzk], [kkk], scale=-1.0)
                ts("dve", kk[:], kk[:], omlt[:, l, h:h + 1], None, ALU.mult, ALU.bypass, [kkk, ("omlt", l)], [kkk])
                lf, lfk = sg, sgk
                act(lf[:], sg[:], AF.Ln, [sgk, ("omlt", l), ("lbt", l)], [lfk], scale=omlt[:, l, h:h + 1], bias=lbt[:, l, h:h + 1])
                bb, bbk = f512.next()
                P.add("dve", lambda e, bb=bb, lf=lf: e.tensor_tensor_scan(out=bb[:], data0=rmask, data1=lf[:], initial=0.0,
                                                                           op0=ALU.mult, op1=ALU.add), [lfk, "cst"], [bbk])
                e1, e1k = f512.next()
                act(e1[:], bb[:], AF.Exp, [bbk], [e1k])
                e2, e2k = bb, bbk
                act(e2[:], bb[:], AF.Exp, [bbk, e1k], [e2k], scale=-1.0)
                qt_, qtk = b512.next()
                tt("dve", qt_[:], qs[:], e1[:], ALU.mult, [qsk, e1k], [qtk])
                kt_, ktk = b512.next()
                tt("dve", kt_[:], kk[:], e2[:], ALU.mult, [kkk, e2k], [ktk])
                kh, khk = b512.next()
                ebl = e1[:].rearrange("p (j t) -> p j t", t=64)[:, :, 63:64]
                tt("dve", kh[:].rearrange("p (j t) -> p j t", t=64), kt_[:].rearrange("p (j t) -> p j t", t=64),
                   ebl.to_broadcast([128, 8, 64]), ALU.mult, [ktk, e1k], [khk])
                vt, vk = vtok.next()
                for half in range(2):
                    pv, pvk = pmm.next()
                    for cc in range(4):
                        ch = half * 4 + cc
                        for c in range(8):
                            mm(pv[0:64, cc * 128:(cc + 1) * 128], aT[:, c, 2 + s * 512 + ch * 64: 2 + s * 512 + (ch + 1) * 64],
                               wt[:, c, 256:384], c == 0, c == 7, [wk] + ar, [pvk])
                    cp("act", vt[:, half * 4:(half + 1) * 4, 0:128], pv[0:64, :].rearrange("p (c v) -> p c v", c=4), [pvk], [vk])
                kT_t, kTk = ktok.next()
                for ch in range(8):
                    tr(ptr[0:64, ch * 128:(ch + 1) * 128], kh[:, ch * 64:(ch + 1) * 64], identb, [khk, "cbf"], ["ptr"])
                cp("act", kT_t[:], ptr[0:64, :].rearrange("p (c v) -> p c v", c=8), ["ptr"], [kTk])
                po, pok = pmm.next()
                for ch in range(8):
                    cs = slice(ch * 64, (ch + 1) * 64)
                    apt, ak = psm.next()
                    a_ps = apt[0:64, 0:64]
                    mm(a_ps, kt_[:, cs], qt_[:, cs], True, True, [ktk, qtk], [ak])
                    am, amk = s64.next()
                    tt("dve", am[:], a_ps, maskT, ALU.mult, [ak, "cst"], [amk])
                    if ch == 0 and s == 0:
                        cp("act", Sbf[:], S_h, [SK], ["Sbf"])
                    mm(po[:, cs], vt[:, ch, 0:128], am[:], True, False, [vk, amk], [pok])
                    mm(po[:, cs], Sbf[:], qt_[:, cs], False, True, ["Sbf", qtk], [pok])
                    kvt, kvk = psm.next()
                    kv_ps = kvt[:, 0:128]
                    mm(kv_ps, kT_t[:, ch, :], vt[:, ch, 0:128], True, True, [kTk, vk], [kvk])
                    stt(S_h, S_h, e1[:, ch * 64 + 63: ch * 64 + 64], kv_ps, ALU.mult, ALU.add, [SK, e1k, kvk], [SK])
                    cp("act", Sbf[:], S_h, [SK], ["Sbf"])
                head_norm_gate(po[:], pok, gs, gsk, l, h, s)
        out_proj(hgout_d[j], "hgo")

    def mlstm(l):
        j = l // 2
        w_in = mlin_d[j]
        rmsnorm_to_aT(O_GMIX, l)
        B_, U_, MG, M_, WI, CL, LI, LF = (g8[k] for k in ("B", "u", "Mg", "m", "wi", "cl", "li", "lf"))
        gw, gwk = wload(wr, [(lambda t: t[:, :, 0:16], rows(w_in)[:, :, 3072:3088])])
        cp("dve", B_[:, 0:1], mlcar[:, j, 0:1], ["mlcar"], ["g8B"])
        cp("dve", MG[:, 0:1], mlcar[:, j, 1:2], ["mlcar"], ["g8Mg"])
        for s in range(NS):
            ar = aT_reads(s)
            pi, pik = pmm.next()
            pf, pfk = pmm.next()
            for c in range(8):
                mm(pi[0:8, :], gw[:, c, 0:8], aTs(c, s), c == 0, c == 7, [gwk] + ar, [pik])
            for c in range(8):
                mm(pf[0:8, :], gw[:, c, 8:16], aTs(c, s), c == 0, c == 7, [gwk] + ar, [pfk])
            csl = slice(1 + s * 512, 1 + (s + 1) * 512)
            act(LI[:, csl], pi[0:8, :], AF.Identity, [pik, "vecs"], ["g8li"], bias=vecs[0:8, l * NV_L + O_BI: l * NV_L + O_BI + 1])
            act(LF[:, csl], pf[0:8, :], AF.Sigmoid, [pfk, "vecs"], ["g8lf"], bias=vecs[0:8, l * NV_L + O_BF: l * NV_L + O_BF + 1])
            act(LF[:, csl], LF[:, csl], AF.Ln, ["g8lf"], ["g8lf"])
        onesT = g8["wi"]
        P.add("dve", lambda e: e.memset(onesT[:, :], 1.0), [], ["g8wi"])
        P.add("dve", lambda e: e.tensor_tensor_scan(out=B_[:, 1:T + 1], data0=onesT[:, 1:T + 1], data1=LF[:, 1:T + 1],
                                                     initial=B_[:, 0:1], op0=ALU.mult, op1=ALU.add),
              ["g8lf", "g8wi", "g8B"], ["g8B"])
        cp("dve", mlcar[:, j, 0:1], B_[:, T:T + 1], ["g8B"], ["mlcar"])
        tt("dve", U_[:, 1:T + 1], LI[:, 1:T + 1], B_[:, 1:T + 1], ALU.subtract, ["g8li", "g8B"], ["g8li"])
        P.add("dve", lambda e: e.tensor_tensor_scan(out=MG[:, 1:T + 1], data0=U_[:, 1:T + 1], data1=U_[:, 1:T + 1],
                                                     initial=MG[:, 0:1], op0=ALU.max, op1=ALU.max),
              ["g8li", "g8Mg"], ["g8Mg"])
        tt("dve", M_[:, 1:T + 1], B_[:, 1:T + 1], MG[:, 1:T + 1], ALU.add, ["g8B", "g8Mg"], ["g8B"])
        act(CL[:, 1:T + 1], M_[:, 1:T + 1], AF.Exp, ["g8B"], ["g8B"], scale=-1.0)
        mgp = MG[:, 0:T].rearrange("p (j t) -> p j t", t=64)[:, :, 0:1]
        tt("dve", WI[:, 1:T + 1].rearrange("p (j t) -> p j t", t=64), mgp.to_broadcast([8, NCH, 64]),
           MG[:, 1:T + 1].rearrange("p (j t) -> p j t", t=64), ALU.subtract, ["g8Mg", "g8wi"], ["g8wi"])
        act(WI[:, 1:T + 1], WI[:, 1:T + 1], AF.Exp, ["g8wi"], ["g8wi"])
        cp("dve", mlcar[:, j, 1:2], MG[:, T:T + 1], ["g8Mg"], ["mlcar"])
        for g in range(NCH // 8):
            pbc, pbk = psm.next()
            for cc in range(8):
                ch = g * 8 + cc
                tr(pbc[0:64, cc * 8:(cc + 1) * 8], U_[:, 1 + ch * 64: 1 + (ch + 1) * 64], ident[0:8, 0:8], ["g8li", "cst"], [pbk])
            cp("act", uT[:, g * 8:(g + 1) * 8, :], pbc[0:64, 0:64].rearrange("p (c h) -> p c h", c=8), [pbk], ["uT"])

        def load_head(h):
            return wload(wr, [
                (lambda t: t[:, :, 0:64], rows(w_in)[:, :, h * 64:(h + 1) * 64]),
                (lambda t: t[:, :, 64:128], rows(w_in)[:, :, 512 + h * 64: 512 + (h + 1) * 64]),
                (lambda t: t[:, :, 128:256], rows(w_in)[:, :, 1024 + h * 128: 1024 + (h + 1) * 128]),
                (lambda t: t[:, :, 256:384], rows(w_in)[:, :, 2048 + h * 128: 2048 + (h + 1) * 128]),
            ])
        nxt = load_head(0)
        for h in range(8):
            wt, wk = nxt
            if h + 1 < 8:
                nxt = load_head(h + 1)
            Cn = Cst[:, j, h, :]
            CK = ("Cst", j, h)
            for s in range(NS):
                ar = aT_reads(s)
                csl = slice(1 + s * 512, 1 + (s + 1) * 512)
                pq, pqk = pmm.next()
                pk_, pkk = pmm.next()
                pg, pgk = pmm.next()
                for c in range(8):
                    mm(pq[0:64, :], wt[:, c, 0:64], aTs(c, s), c == 0, c == 7, [wk] + ar, [pqk])
                for c in range(8):
                    mm(pk_[0:64, :], wt[:, c, 64:128], aTs(c, s), c == 0, c == 7, [wk] + ar, [pkk])
                for c in range(8):
                    mm(pg[:], wt[:, c, 256:384], aTs(c, s), c == 0, c == 7, [wk] + ar, [pgk])
                gs, gsk = f512.next()
                act(gs[:], pg[:], AF.Sigmoid, [pgk], [gsk])
                kTs, kTk = b512.next()
                act(kTs[0:64, :], pk_[0:64, :], AF.Copy, [pkk], [kTk], scale=0.125)
                selh = sel[:, h * 128:(h + 1) * 128]
                pbc, pbk = psm.next()
                mm(pbc[:], selh, MG[:, csl], True, True, ["g8Mg", "cst"], [pbk])
                mgb, mgbk = f512.next()
                mgp_t, mgpk = f512.next()
                cp("act", mgp_t[0:64, :], pbc[0:64, :], [pbk], [mgpk])
                tt("dve", mgb[0:64, :].rearrange("p (j t) -> p j t", t=64), negmT.unsqueeze(1).to_broadcast([64, 8, 64]),
                   pbc[0:64, :].rearrange("p (j t) -> p j t", t=64), ALU.subtract, [pbk, "cst"], [mgbk])
                pbc, pbk = psm.next()
                mm(pbc[:], selh, WI[:, csl], True, True, ["g8wi", "cst"], [pbk])
                wib, wibk = f512.next()
                cp("act", wib[0:64, :], pbc[0:64, :], [pbk], [wibk])
                qs, qsk = b512.next()
                cp("act", qs[0:64, :], pq[0:64, :], [pqk], [qsk])
                qw, qwk = b512.next()
                tt("dve", qw[0:64, :], pq[0:64, :], wib[0:64, :], ALU.mult, [pqk, wibk], [qwk])
                pbc, pbk = psm.next()
                mm(pbc[:], selh, CL[:, csl], True, True, ["g8B", "cst"], [pbk])
                clb, clbk = f512.next()
                cp("act", clb[:], pbc[:], [pbk], [clbk])
                vt, vk = vtok.next()
                kt_t, ktk = ktok.next()
                for half in range(2):
                    pv, pvk = pmm.next()
                    for cc in range(4):
                        ch = half * 4 + cc
                        for c in range(8):
                            mm(pv[0:64, cc * 128:(cc + 1) * 128], aT[:, c, 2 + s * 512 + ch * 64: 2 + s * 512 + (ch + 1) * 64],
                               wt[:, c, 128:256], c == 0, c == 7, [wk] + ar, [pvk])
                    cp("act", vt[:, half * 4:(half + 1) * 4, 0:128], pv[0:64, :].rearrange("p (c v) -> p c v", c=4), [pvk], [vk])
                pkt, pktk = pmm.next()
                for ch in range(8):
                    for c in range(8):
                        mm(pkt[0:64, ch * 64:(ch + 1) * 64], aT[:, c, 2 + s * 512 + ch * 64: 2 + s * 512 + (ch + 1) * 64],
                           wt[:, c, 64:128], c == 0, c == 7, [wk] + ar, [pktk])
                pnum, pnk = pmm.next()
                pden, pdk = pmm.next()
                for ch in range(8):
                    gch = s * 8 + ch
                    cs = slice(ch * 64, (ch + 1) * 64)
                    stt_, sk_ = psm.next()
                    st_ps = stt_[0:64, 0:64]
                    mm(st_ps, kTs[0:64, cs], qs[0:64, cs], True, True, [kTk, qsk], [sk_])
                    dm, dmk = d64.next()
                    act(dm[:], mgb[0:64, cs], AF.Exp, [mgbk, "uT"], [dmk], bias=uT[:, gch, h:h + 1])
                    sT, sTk = s64.next()
                    tt("dve", sT[:], st_ps, dm[:], ALU.mult, [sk_, dmk], [sTk])
                    if ch == 0 and s == 0:
                        cp("act", Cbf[:], Cn[:, 0:128], [CK], ["Cbf"])
                        act(nBf[:], cst[0:64, 0:128], AF.Identity, [CK, "cst"], ["nBf"], scale=0.0, bias=Cn[:, 128:129])
                    mm(pnum[:, cs], vt[:, ch, 0:128], sT[:], True, False, [vk, sTk], [pnk])
                    mm(pnum[:, cs], Cbf[:], qw[0:64, cs], False, True, ["Cbf", qwk], [pnk])
                    mm(pden[:, cs], onesb[0:64, :], sT[:], True, False, ["cbf", sTk], [pdk])
                    mm(pden[:, cs], nBf[:], qw[0:64, cs], False, True, ["nBf", qwk], [pdk])
                    e_ = ch * 64 + 63
                    w_t, w_k = sm.next()
                    act(w_t[0:64, 0:1], mgp_t[0:64, e_:e_ + 1], AF.Exp, [mgpk, "uT"], [w_k], scale=-1.0, bias=uT[:, gch, h:h + 1])
                    kw, kwk = s64.next()
                    ts("dve", kw[:], pkt[0:64, cs], w_t[0:64, 0:1], 0.125, ALU.mult, ALU.mult, [pktk, w_k], [kwk])
                    cnt_, cnk0 = psm.next()
                    cn_ps = cnt_[0:64, 0:129]
                    cnk = [cnk0]
                    mm(cn_ps, kw[:], vt[:, ch, 0:129], True, True, [kwk, vk], cnk)
                    stt(Cn, Cn, wib[0:64, e_:e_ + 1], cn_ps, ALU.mult, ALU.add, [CK, wibk] + cnk, [CK])
                    cp("act", Cbf[:], Cn[:, 0:128], [CK], ["Cbf"])
                    act(nBf[:], cst[0:64, 0:128], AF.Identity, [CK, "cst"], ["nBf"], scale=0.0, bias=Cn[:, 128:129])
                dd, ddk = f512.next()
                act(dd[:], pden[:], AF.Abs, [pdk], [ddk])
                tt("dve", dd[:], dd[:], clb[:], ALU.max, [ddk, clbk], [ddk])
                recip(dd[:], dd[:], [ddk], [ddk])
                hh, hhk = f512.next()
                tt("dve", hh[:], pnum[:], dd[:], ALU.mult, [pnk, ddk], [hhk])
                head_norm_gate(hh[:], hhk, gs, gsk, l, h, s)
        out_proj(mlout_d[j], "mlo")

    def xattn(l):
        rmsnorm_to_aT(O_GXA, l)
        for c in range(8):
            ts("dve", memnT[:, c, :], memhT[:, c, :], vcol(l, O_GMEM + c), None, ALU.mult, ALU.bypass,
               ["memhT", "vecs"], ["memnT"])
        for hf in range(2):
            wt, wk = wload(wr, [(lambda t: t[:], rows(wkv_d[l])[:, :, hf * 512:(hf + 1) * 512])])
            for m4 in range(4):
                pt, pk = pmm.next()
                for c in range(8):
                    mm(pt[:, 0:NMEM], wt[:, c, m4 * 128:(m4 + 1) * 128], memnT[:, c, :], c == 0, c == 7, [wk, "memnT"], [pk])
                cp("act", KT[:, hf * 4 + m4, :], pt[:, 0:NMEM], [pk], ["KT"])
        for hf in range(2):
            wt, wk = wload(wr, [(lambda t: t[:], rows(wkv_d[l])[:, :, D + hf * 512: D + (hf + 1) * 512])])
            for kt in range(2):
                pt, pk = pmm.next()
                for c in range(8):
                    mm(pt[:], memnT[:, c, kt * 128:(kt + 1) * 128], wt[:, c, :], c == 0, c == 7, [wk, "memnT"], [pk])
                cp("act", Vt[:, kt, hf * 512:(hf + 1) * 512], pt[:], [pk], ["Vt"])
        for hp in range(2):
            wt, wk = wload(wr, [(lambda t: t[:], rows(wq_d[l])[:, :, hp * 512:(hp + 1) * 512])])
            for hh_ in range(2):
                hd = hp * 2 + hh_
                for s in range(NS):
                    ar = aT_reads(s)
                    q0, q0k = qx.next()
                    q1, q1k = qx.next()
                    qts = ((q0, q0k), (q1, q1k))
                    for dc in range(2):
                        pt, pk = pmm.next()
                        for c in range(8):
                            mm(pt[:], wt[:, c, hh_ * 256 + dc * 128: hh_ * 256 + (dc + 1) * 128], aTs(c, s), c == 0, c == 7,
                               [wk] + ar, [pk])
                        act(qts[dc][0][:], pt[:], AF.Copy, [pk], [qts[dc][1]], scale=1.0 / 16.0)
                    po0, po0k = pmm.next()
                    po1, po1k = pmm.next()
                    pos = ((po0, po0k), (po1, po1k))
                    for tt_ in range(4):
                        tsl = slice(tt_ * 128, (tt_ + 1) * 128)
                        sct, sck = psm.next()
                        sc = sct[:, 0:256]
                        scks = [sck]
                        for dc in range(2):
                            mm(sc, qts[dc][0][:, tsl], KT[:, hd * 2 + dc, :], dc == 0, dc == 1, [qts[dc][1], "KT"], scks)
                        st_, sk = sm.next()
                        P.add("dve", lambda e, st_=st_, sc=sc: e.tensor_reduce(out=st_[:, 0:1], in_=sc, axis=AX.X, op=ALU.max, negate=True),
                              scks, [sk])
                        ee, eek = f512.next()
                        act(ee[:, 0:256], sc, AF.Exp, scks + [sk], [eek, sk], bias=st_[:, 0:1], accum=st_[:, 1:2])
                        recip(st_[:, 2:3], st_[:, 1:2], [eek, sk], [sk])
                        pp, ppk = b512.next()
                        ts("dve", pp[:, 0:256], ee[:, 0:256], st_[:, 2:3], None, ALU.mult, ALU.bypass, [eek, sk], [ppk])
                        for kt in range(2):
                            tr(ptr[:, (tt_ % 2) * 256 + kt * 128:(tt_ % 2) * 256 + (kt + 1) * 128], pp[:, kt * 128:(kt + 1) * 128], identb,
                               [ppk, "cbf"], ["ptr"])
                        pT, pTk = b512.next()
                        cp("act", pT[:, 0:256], ptr[:, (tt_ % 2) * 256:(tt_ % 2) * 256 + 256], ["ptr"], [pTk])
                        for dc in range(2):
                            for kt in range(2):
                                mm(pos[dc][0][:, tsl], Vt[:, kt, hd * 256 + dc * 128: hd * 256 + (dc + 1) * 128],
                                   pT[:, kt * 128:(kt + 1) * 128], kt == 0, kt == 1, ["Vt", pTk], [pos[dc][1]])
                    for dc in range(2):
                        cp("act", oT[:, hd * 2 + dc, s * 512:(s + 1) * 512], pos[dc][0][:], [pos[dc][1]], [("oT", hd * 2 + dc, s)])
        out_proj(wo_d[l], "xo")

    nsub = -(-T // 510)
    bnds = [round(i * T / nsub) for i in range(nsub + 1)]

    def ffn(l, wave):
        rmsnorm_to_aT(O_GFFN, l)
        cp("dve", aT[:, :, 0:2], ffhalo[:, l, :, :], ["ffhalo"], [("aTh",)])
        cp("dve", ffhalo[:, l, :, :], aT[:, :, T:T + 2], [("aT", c, NS - 1) for c in range(8)], ["ffhalo"])

        def load_g(g):
            return wload(wr, [(lambda t: t[:, :, 0:256], rows(wup_d[l])[:, :, g * 256:(g + 1) * 256]),
                              (lambda t: t[:, :, 256:512], rows(wup_d[l])[:, :, DFF + g * 256: DFF + (g + 1) * 256])])
        nxt = load_g(0)
        for g in range(NFF // 2):
            wt, wk = nxt
            if g + 1 < NFF // 2:
                nxt = load_g(g + 1)
            for jj in range(2):
                jf = g * 2 + jj
                for si in range(nsub):
                    a, b = bnds[si], bnds[si + 1]
                    n = b - a
                    rd = [wk, ("aTh",)] + [("aT", c, s) for c in range(8) for s in range(NS)]
                    pgt, pgk = pmm.next()
                    pvt, pvk = pmm.next()
                    for c in range(8):
                        mm(pgt[:, 0:n + 2], wt[:, c, jj * 128:(jj + 1) * 128], aT[:, c, a:b + 2], c == 0, c == 7, rd, [pgk])
                    for c in range(8):
                        mm(pvt[:, 0:n + 2], wt[:, c, 256 + jj * 128: 256 + (jj + 1) * 128], aT[:, c, a:b + 2], c == 0, c == 7, rd, [pvk])
                    outs = []
                    for (pt, pk, col) in ((pgt, pgk, jf), (pvt, pvk, NFF + jf)):
                        y, yk = f512.next()
                        act(y[:, 0:n], pt[:, 2:n + 2], AF.Identity, [pk, "vecs"], [yk],
                            scale=vcol(l, O_CW2 + col), bias=vcol(l, O_CB + col))
                        stt(y[:, 0:n], pt[:, 1:n + 1], vcol(l, O_CW1 + col), y[:, 0:n], ALU.mult, ALU.add, [pk, yk, "vecs"], [yk])
                        stt(y[:, 0:n], pt[:, 0:n], vcol(l, O_CW0 + col), y[:, 0:n], ALU.mult, ALU.add, [pk, yk, "vecs"], [yk])
                        outs.append((y, yk))
                    (yg, ygk), (yv, yvk) = outs
                    act(yg[:, 0:n], yg[:, 0:n], AF.Silu, [ygk], [ygk])
                    tt("dve", prod[:, jf, a:b], yg[:, 0:n], yv[:, 0:n], ALU.mult, [ygk, yvk], [("prod", jf)])
        def load_d(mp):
            return wload(wdr, [(lambda t: t[:], wdn_d[l].rearrange("(j p) n -> p j n", p=128)[:, :, mp * 128:(mp + 1) * 128])])
        nxt = load_d(0)
        for mp in range(8):
            wt, wk = nxt
            if mp + 1 < 8:
                nxt = load_d(mp + 1)
            for m2 in range(1):
                m = mp
                for s in range(NS):
                    pt, pk = pmm.next()
                    for jf in range(NFF):
                        mm(pt[:], wt[:, jf, m2 * 128:(m2 + 1) * 128], prod[:, jf, s * 512:(s + 1) * 512], jf == 0, jf == NFF - 1,
                           [wk, ("prod", jf)], [pk])
                    tt("dve", hT[:, m, s * 512:(s + 1) * 512], hT[:, m, s * 512:(s + 1) * 512], pt[:], ALU.add,
                       [pk, ("hT", m, s)], [("hT", m, s)])

    olanes = []
    for w in range(NW):
        for t4 in range(T // 128):
            s = t4 // 4
            xt, xk = xin.next()
            r0 = w * T + t4 * 128
            P.dma("sp", xt[:], x_d[r0:r0 + 128, :], writes=[xk], lane=xk)
            for g in range(2):
                pt, pk = pmm.next()
                for c4 in range(4):
                    c = g * 4 + c4
                    tr(pt[:, c4 * 128:(c4 + 1) * 128], xt[:, c * 128:(c + 1) * 128], ident, [xk, "cst"], [pk])
                cp("act", hT[:, g * 4:(g + 1) * 4, t4 * 128:(t4 + 1) * 128], pt[:].rearrange("p (c t) -> p c t", c=4), [pk],
                   [("hT", c, s) for c in range(g * 4, g * 4 + 4)])
        for l in layers:
            if l % 2 == 0:
                hgrn2(l)
            else:
                mlstm(l)
            xattn(l)
            ffn(l, w)
        for s in range(NS):
            sl = slice(s * 512, (s + 1) * 512)
            if final:
                pt, pk = pmm.next()
                for c in range(8):
                    bt, bk = b512.next()
                    act(bt[:], hT[:, c, sl], AF.Square, [("hT", c, s)], [bk])
                    mm(pt[:], onesb, bt[:], c == 0, c == 7, [bk, "cbf"], [pk])
                rt, rk = f512.next()
                act(rt[:], pt[:], AF.Sqrt, [pk, "vecs"], [rk], scale=1.0 / D, bias=EPSC)
                recip(rt[:], rt[:], [rk], [rk])
            for t4 in range(4):
                tsl = slice(s * 512 + t4 * 128, s * 512 + (t4 + 1) * 128)
                xt, xk = xin.next()
                for g in range(2):
                    pt2, pk2 = pmm.next()
                    for c4 in range(4):
                        c = g * 4 + c4
                        if final:
                            yt, yk = f512.next()
                            stt(yt[:, 0:128], hT[:, c, tsl], vecs[:, DEPTH * NV_L + c: DEPTH * NV_L + c + 1], rt[:, t4 * 128:(t4 + 1) * 128], ALU.mult, ALU.mult,
                                [("hT", c, s), rk, "vecs"], [yk])
                            tr(pt2[:, c4 * 128:(c4 + 1) * 128], yt[:, 0:128], ident, [yk, "cst"], [pk2])
                        else:
                            tr(pt2[:, c4 * 128:(c4 + 1) * 128], hT[:, c, tsl], ident, [("hT", c, s), "cst"], [pk2])
                    cp("act", xt[:, g * 512:(g + 1) * 512], pt2[:], [pk2], [xk])
                r0 = w * T + s * 512 + t4 * 128
                ln = ("o", xk)
                P.dma("sp", out_d[r0:r0 + 128, :], xt[:], reads=[xk], writes=[("outrow", r0)], lane=ln)
                if ln not in olanes:
                    olanes.append(ln)
    P.finish(final_lanes=olanes)
    return nc, P.stats


def _consts():
    cst = np.zeros((128, 128 + 64 + 64 + 512 + 1024), np.float32)
    cst[:, 0:128] = np.eye(128, dtype=np.float32)
    s = np.arange(64)[:, None]
    t = np.arange(64)[None, :]
    cst[0:64, 128:192] = (s <= t).astype(np.float32)
    cst[0:64, 192:256] = np.where(s <= t, 0.0, -1e30).astype(np.float32)
    rm = np.ones(512, np.float32)
    rm[::64] = 0.0
    cst[:, 256:768] = rm[None, :]
    for h in range(8):
        cst[h, 768 + h * 128: 768 + (h + 1) * 128] = 1.0
    cbf = np.zeros((128, 256), np.float32)
    cbf[:, 0:128] = np.eye(128, dtype=np.float32)
    cbf[:, 128:256] = 1.0
    return cst, cbf.astype(ml_dtypes.bfloat16)


def _cols(v, n):
    return np.ascontiguousarray(np.asarray(v, np.float32).reshape(n, 128).T)


def _pack_vecs(inp):
    vec = np.zeros((128, DEPTH * NV_L + 16), np.float32)
    for l in range(DEPTH):
        b = l * NV_L
        j = l // 2
        vec[:, b + 0:b + 8] = _cols(inp["norm_mix_g"][l], 8)
        vec[:, b + 8:b + 16] = _cols(inp["norm_xa_g"][l], 8)
        vec[:, b + 16:b + 24] = _cols(inp["norm_mem_g"][l], 8)
        vec[:, b + 24:b + 32] = _cols(inp["norm_ffn_g"][l], 8)
        vec[:, b + 32:b + 76] = _cols(inp["ffn_conv_w"][l][0], 44)
        vec[:, b + 76:b + 120] = _cols(inp["ffn_conv_w"][l][1], 44)
        vec[:, b + 120:b + 164] = _cols(inp["ffn_conv_w"][l][2], 44)
        vec[:, b + 164:b + 208] = _cols(inp["ffn_conv_b"][l], 44)
        vec[:, b + 208:b + 216] = _cols((inp["hg_norm_g"] if l % 2 == 0 else inp["ml_norm_g"])[j], 8)
        vec[:, b + 216:b + 224] = _cols(inp["hg_lb_logits"][l], 8)
        if l % 2 == 1:
            vec[0:8, b + 224] = np.asarray(inp["ml_b_gate"][j][0:8], np.float32)
            vec[0:8, b + 225] = np.asarray(inp["ml_b_gate"][j][8:16], np.float32)
    vec[:, DEPTH * NV_L: DEPTH * NV_L + 8] = _cols(inp["final_g"], 8)
    vec[:, DEPTH * NV_L + 8] = EPS
    return vec


_CACHE = {}
MODE = "fused"
WAVE_T = 512

_WNAMES = ("hg_w_in", "hg_w_out", "ml_w_in", "ml_w_out", "xa_wq", "xa_wkv", "xa_wo", "ffn_w_up", "ffn_w_down")


def _get_nc(**kw):
    key = tuple(sorted(kw.items()))
    if key not in _CACHE:
        _CACHE[key] = build(**kw)[0]
    return _CACHE[key]


def _launch(nc, xs, inp, vec, cst, cbf, n):
    shared = {k: np.ascontiguousarray(inp[k], dtype=np.float32) for k in _WNAMES}
    in_maps = []
    for b in range(n):
        m = dict(shared)
        m["x"] = np.ascontiguousarray(xs[b], dtype=np.float32)
        m["mem"] = np.ascontiguousarray(inp["mem"][b], dtype=np.float32)
        m["vecs"] = vec
        m["cst"] = cst
        m["cbf"] = cbf
        in_maps.append(m)
    res = run_bass_kernel_spmd(nc, in_maps, core_ids=list(range(n)))
    return [r["out"] for r in res.results]


def kernel(**inputs):
    inp = {k: np.asarray(v) for k, v in inputs.items()}
    vec = _pack_vecs(inp)
    cst, cbf = _consts()
    x = inp["x"]
    n = x.shape[0]
    S = x.shape[1]
    if MODE == "fused":
        nc = _get_nc(S=S, T=WAVE_T, layers=(0, 1, 2, 3), final=True)
        outs = _launch(nc, [x[b] for b in range(n)], inp, vec, cst, cbf, n)
    else:
        hs = [x[b] for b in range(n)]
        for l in range(DEPTH):
            nc = _get_nc(S=S, T=WAVE_T, layers=(l,), final=(l == DEPTH - 1))
            hs = _launch(nc, hs, inp, vec, cst, cbf, n)
        outs = hs
    return np.stack(outs, axis=0).astype(np.float32)
```

```python
import numpy as np
import ml_dtypes
import concourse.bass as bass
import concourse.mybir as mybir
from concourse.bass_utils import run_bass_kernel_spmd
from contextlib import ExitStack

F32 = mybir.dt.float32
BF16 = mybir.dt.bfloat16
AF = mybir.ActivationFunctionType
ALU = mybir.AluOpType
AX = mybir.AxisListType

ENGS = ("pe", "act", "dve", "pool", "sp")

D = 1024
SEQ = 4096
NMEM = 256
DEPTH = 4
DFF = 2816
NFF = DFF // 128
EPS = 1e-6
HG_IN = 4096
ML_IN = 3088
NV_L = 232


class Prog:
    def __init__(self, nc):
        self.nc = nc
        self.ops = []
        self.stack = ExitStack()
        self.nt = 0
        self.psum_keys = set()

    def sb(self, shape, dtype, name=None):
        self.nt += 1
        return self.stack.enter_context(self.nc.sbuf_tensor("sb_" + (name or f"t{self.nt}"), list(shape), dtype))

    def ps(self, shape, dtype, name=None):
        self.nt += 1
        return self.stack.enter_context(self.nc.psum_tensor("ps_" + (name or f"p{self.nt}"), list(shape), dtype))

    def add(self, eng, fn, reads=(), writes=(), lane=None):
        self.ops.append((eng, fn, tuple(reads), tuple(writes), lane))

    def dma(self, q, out, in_, reads=(), writes=(), lane=None):
        assert lane is not None
        self.ops.append((q, (lambda e, o=out, i=in_: e.dma_start(out=o, in_=i)), tuple(reads), tuple(writes), lane))

    def finish(self, final_lanes=()):
        nc = self.nc
        ops = self.ops
        n = len(ops)
        last_w = {}
        readers = {}
        know = {e: {} for e in ENGS}
        vc = [None] * n
        waits = [None] * n
        signaled = [False] * n
        lane_cnt = {}
        dma_cnt = [0] * n
        for i, (eng, fn, reads, writes, lane) in enumerate(ops):
            deps = set()
            raw = set()
            for r in reads:
                j = last_w.get(r)
                if j is not None:
                    deps.add(j)
                    raw.add(j)
                if r in self.psum_keys:
                    for j in readers.get(r, ()):
                        deps.add(j)
            for w in writes:
                j = last_w.get(w)
                if j is not None:
                    deps.add(j)
                for j in readers.get(w, ()):
                    deps.add(j)
            kn = know[eng]
            wl = []
            for j in sorted(deps):
                je, _, _, _, jl = ops[j]
                if jl is not None:
                    key = ("L", jl)
                    val = dma_cnt[j]
                else:
                    if je == eng and lane is None and (eng == "pe" or j not in raw):
                        continue
                    key = je
                    val = j
                if kn.get(key, -1) >= val:
                    continue
                wl.append(j)
                if jl is None:
                    signaled[j] = True
                for k2, v2 in vc[j].items():
                    if kn.get(k2, -1) < v2:
                        kn[k2] = v2
            waits[i] = wl
            v = dict(kn)
            if lane is not None:
                c = lane_cnt.get(lane, 0) + 1
                lane_cnt[lane] = c
                dma_cnt[i] = c
                v[("L", lane)] = c
            else:
                v[eng] = i
            vc[i] = v
            for r in reads:
                readers.setdefault(r, []).append(i)
            for w in writes:
                last_w[w] = i
                readers[w] = []
        sig = [0] * n
        cnt = {e: 0 for e in ENGS}
        for i, (eng, fn, reads, writes, lane) in enumerate(ops):
            if lane is None and signaled[i]:
                cnt[eng] += 1
                sig[i] = cnt[eng]
        self.stats = dict(n_ops=n, signals=dict(cnt), lanes=len(lane_cnt),
                          n_waits=sum(len(w) for w in waits),
                          per_eng={e: sum(1 for o in ops if o[0] == e) for e in ENGS})
        st = self.stack
        esem = {e: st.enter_context(nc.semaphore(f"s_{e}")) for e in ENGS}
        lsem = {l: st.enter_context(nc.semaphore(f"l_{k}")) for k, l in enumerate(lane_cnt)}
        per_eng = {e: [] for e in ENGS}
        for i, op in enumerate(ops):
            per_eng[op[0]].append(i)

        def emit(eng_name, e):
            for i in per_eng[eng_name]:
                _, fn, _, _, lane = ops[i]
                for j in waits[i]:
                    je, _, _, _, jl = ops[j]
                    if jl is not None:
                        e.wait_ge(lsem[jl], 16 * dma_cnt[j])
                    else:
                        e.wait_ge(esem[je], sig[j])
                ins = fn(e)
                if lane is not None:
                    ins.then_inc(lsem[lane], 16)
                elif signaled[i]:
                    ins.then_inc(esem[eng_name], 1)
            if eng_name == "sp":
                for l in final_lanes:
                    e.wait_ge(lsem[l], 16 * lane_cnt[l])

        with nc.Block() as block:
            @block.tensor
            def _(e):
                emit("pe", e)

            @block.scalar
            def _(e):
                emit("act", e)

            @block.vector
            def _(e):
                emit("dve", e)

            @block.gpsimd
            def _(e):
                emit("pool", e)

            @block.sync
            def _(e):
                emit("sp", e)
        st.close()


class Ring:
    def __init__(self, P, name, n, shape, dtype, psum=False):
        self.name = name
        self.n = n
        self.i = 0
        self.tiles = [(P.ps if psum else P.sb)(shape, dtype, f"{name}{k}") for k in range(n)]
        if psum:
            for k in range(n):
                P.psum_keys.add((name, k))

    def next(self):
        k = self.i % self.n
        self.i += 1
        return self.tiles[k], (self.name, k)


def build(S=SEQ, T=1024, layers=(0, 1, 2, 3), final=True, phases="mxf"):
    nc = bass.Bass("TRN2", target_bir_lowering=False)
    P = Prog(nc)
    NW = S // T
    NS = T // 512
    NCH = T // 64
    L0 = layers[0]

    def din(name, shape, dt=F32):
        return nc.dram_tensor(name, list(shape), dt, kind="ExternalInput").ap()

    x_d = din("x", [S, D])
    mem_d = din("mem", [NMEM, D])
    vec_d = din("vecs", [128, DEPTH * NV_L + 16])
    cst_d = din("cst", [128, 128 + 64 + 64 + 512 + 1024])
    cbf_d = din("cbf", [128, 256], BF16)
    hgin_d = din("hg_w_in", [2, D, HG_IN])
    hgout_d = din("hg_w_out", [2, D, D])
    mlin_d = din("ml_w_in", [2, D, ML_IN])
    mlout_d = din("ml_w_out", [2, D, D])
    wq_d = din("xa_wq", [DEPTH, D, D])
    wkv_d = din("xa_wkv", [DEPTH, D, 2 * D])
    wo_d = din("xa_wo", [DEPTH, D, D])
    wup_d = din("ffn_w_up", [DEPTH, D, 2 * DFF])
    wdn_d = din("ffn_w_down", [DEPTH, DFF, D])
    out_d = nc.dram_tensor("out", [S, D], F32, kind="ExternalOutput").ap()

    hT = P.sb([128, 8, T], F32, "hT")
    aT = P.sb([128, 8, T + 2], BF16, "aT")
    oT = P.sb([128, 8, T], BF16, "oT")
    prod = P.sb([128, NFF, T], BF16, "prod")
    vecs = P.sb([128, DEPTH * NV_L + 16], F32, "vecs")
    cst = P.sb([128, 128 + 64 + 64 + 512 + 1024], F32, "cst")
    cbf = P.sb([128, 256], BF16, "cbf")
    lbt = P.sb([128, DEPTH, 8], F32, "lbt")
    omlt = P.sb([128, DEPTH, 8], F32, "omlt")
    memhT = P.sb([128, 8, NMEM], BF16, "memhT")
    memnT = P.sb([128, 8, NMEM], BF16, "memnT")
    KT = P.sb([128, 8, NMEM], BF16, "KT")
    Vt = P.sb([128, 2, D], BF16, "Vt")
    ffhalo = P.sb([128, DEPTH, 8, 2], BF16, "ffhalo")
    Sst = P.sb([128, 2, 8, 128], F32, "Sst")
    Cst = P.sb([64, 2, 8, 129], F32, "Cst")
    mlcar = P.sb([8, 2, 2], F32, "mlcar")

    ident = cst[:, 0:128]
    maskT = cst[0:64, 128:192]
    negmT = cst[0:64, 192:256]
    rmask = cst[:, 256:768]
    sel = cst[0:8, 768:1792]
    identb = cbf[:, 0:128]
    onesb = cbf[:, 128:256]

    def vcol(l, off, n=1):
        return vecs[:, l * NV_L + off: l * NV_L + off + n]
    O_GMIX, O_GXA, O_GMEM, O_GFFN, O_CW0, O_CW1, O_CW2, O_CB, O_MNG, O_LB, O_BI, O_BF = 0, 8, 16, 24, 32, 76, 120, 164, 208, 216, 224, 225

    wr = Ring(P, "w", 3, [128, 8, 512], BF16)
    wdr = Ring(P, "wd", 2, [128, NFF, 128], BF16)
    xin = Ring(P, "xin", 2, [128, D], F32)
    pmm = Ring(P, "pmm", 4, [128, 512], F32, psum=True)
    psm = Ring(P, "psm", 4, [128, 512], F32, psum=True)
    f512 = Ring(P, "f512", 6, [128, 512], F32)
    nrm = Ring(P, "nrm", 3, [128, 512], F32)
    gsr = Ring(P, "gsr", 3, [128, 512], BF16)
    sm8 = Ring(P, "sm8", 8, [128, 8], F32)
    amr = Ring(P, "amr", 2, [64, 512], BF16)
    sallb = Ring(P, "sallb", 4, [128, 8, 128], BF16)
    Sall = P.sb([128, 9, 129], F32, "Sall")
    b512 = Ring(P, "b512", 8, [128, 512], BF16)
    sm = Ring(P, "sm", 8, [128, 4], F32)
    vtok = Ring(P, "vtok", 2, [64, 8, 130], BF16)
    ktok = Ring(P, "ktok", 2, [64, 8, 128], BF16)
    qx = Ring(P, "qx", 4, [128, 512], BF16)
    _g8 = {k: P.sb([8, T + 1], F32, f"g8{k}") for k in ("li", "lf", "B", "Mg", "wi")}
    g8 = dict(li=_g8["li"], u=_g8["li"], lf=_g8["lf"], B=_g8["B"], m=_g8["B"], cl=_g8["B"], Mg=_g8["Mg"], wi=_g8["wi"])
    uT = P.sb([64, NCH, 8], F32, "uT")

    cnt = [0]

    def uid():
        cnt[0] += 1
        return cnt[0]

    def mm(out, lhsT, rhs, start, stop, reads, writes):
        P.add("pe", lambda e: e.matmul(out, lhsT=lhsT, rhs=rhs, start=start, stop=stop), reads, writes)

    def tr(out, in_, idn, reads, writes):
        P.add("pe", lambda e: e.transpose(out, in_, idn), reads, writes)

    def act(out, in_, func, reads, writes, scale=1.0, bias=None, accum=None, eng="act"):
        kw = {}
        if bias is not None:
            kw["bias"] = bias
        if accum is not None:
            kw["accum_out"] = accum
        P.add(eng, lambda e: e.activation(out=out, in_=in_, func=func, scale=scale, **kw), reads, writes)

    def tt(eng, out, in0, in1, op, reads, writes):
        P.add(eng, lambda e: e.tensor_tensor(out=out, in0=in0, in1=in1, op=op), reads, writes)

    def ts(eng, out, in0, s1, s2, op0, op1, reads, writes):
        P.add(eng, lambda e: e.tensor_scalar(out=out, in0=in0, scalar1=s1, scalar2=s2, op0=op0, op1=op1), reads, writes)

    def stt(out, in0, scalar, in1, op0, op1, reads, writes):
        P.add("dve", lambda e: e.scalar_tensor_tensor(out=out, in0=in0, scalar=scalar, in1=in1, op0=op0, op1=op1), reads, writes)

    def cp(eng, out, in_, reads, writes):
        if eng == "act":
            act(out, in_, AF.Copy, reads, writes)
        else:
            P.add(eng, lambda e: e.tensor_copy(out=out, in_=in_), reads, writes)

    def recip(out, in_, reads, writes):
        P.add("dve", lambda e: e.reciprocal(out=out, in_=in_), reads, writes)

    def wload(ring, parts):
        t, k = ring.next()
        for dst_fn, src in parts:
            P.dma("pool", dst_fn(t), src, writes=[k], lane=k)
        return t, k

    def rows(w2d):
        return w2d.rearrange("(c p) n -> p c n", p=128)

    P.dma("sp", vecs[:], vec_d, writes=["vecs"], lane="c0")
    P.dma("sp", cst[:], cst_d, writes=["cst"], lane="c1")
    P.dma("sp", cbf[:], cbf_d, writes=["cbf"], lane="c2")
    CR = ["vecs", "cst", "cbf"]

    ex = P.sb([128, DEPTH, 8], F32, "ex")
    mxl = P.sb([128, 8], F32, "mxl")
    sml = P.sb([128, 8], F32, "sml")
    lg = [vcol(l, O_LB, 8) for l in range(DEPTH)]
    tt("dve", mxl[:], lg[0], lg[1], ALU.max, CR, ["mxl"])
    tt("dve", mxl[:], mxl[:], lg[2], ALU.max, CR + ["mxl"], ["mxl"])
    tt("dve", mxl[:], mxl[:], lg[3], ALU.max, CR + ["mxl"], ["mxl"])
    for l in range(DEPTH):
        tt("dve", ex[:, l, :], lg[l], mxl[:], ALU.subtract, CR + ["mxl"], [("ex", l)])
        act(ex[:, l, :], ex[:, l, :], AF.Exp, [("ex", l)], [("ex", l)])
    tt("dve", sml[:], ex[:, 0, :], ex[:, 1, :], ALU.add, [("ex", 0), ("ex", 1)], ["sml"])
    tt("dve", sml[:], sml[:], ex[:, 2, :], ALU.add, ["sml", ("ex", 2)], ["sml"])
    tt("dve", sml[:], sml[:], ex[:, 3, :], ALU.add, ["sml", ("ex", 3)], ["sml"])
    recip(sml[:], sml[:], ["sml"], ["sml"])
    P.add("dve", lambda e: e.memset(lbt[:, 0, :], 0.0), [], [("lbt", 0)])
    for l in range(1, DEPTH):
        if l == 1:
            cp("dve", lbt[:, 1, :], ex[:, 1, :], [("ex", 1)], [("lbt", 1)])
        else:
            tt("dve", lbt[:, l, :], lbt[:, l - 1, :], ex[:, l, :], ALU.add, [("lbt", l - 1), ("ex", l)], [("lbt", l)])
    for l in range(1, DEPTH):
        pass
    for l in range(DEPTH - 1, 0, -1):
        tt("dve", lbt[:, l, :], lbt[:, l, :], sml[:], ALU.mult, [("lbt", l), "sml"], [("lbt", l)])
    for l in range(DEPTH):
        ts("dve", omlt[:, l, :], lbt[:, l, :], -1.0, 1.0, ALU.mult, ALU.add, [("lbt", l)], [("omlt", l)])

    P.add("pool", lambda e: e.memset(Sst[:], 0.0), [], ["Sst"])
    P.add("pool", lambda e: e.memset(Cst[:], 0.0), [], ["Cst"])
    P.add("pool", lambda e: e.memset(mlcar[:], 0.0), [], ["mlcar"])
    P.add("pool", lambda e: e.memset(ffhalo[:], 0.0), [], ["ffhalo"])
    for k_, r in enumerate(vtok.tiles):
        P.add("pool", lambda e, r=r: e.memset(r[:, :, 128:130], 1.0), [], [("vtok", k_)])

    for kt in range(2):
        xt, xk = xin.next()
        P.dma("sp", xt[:], mem_d[kt * 128:(kt + 1) * 128, :], writes=[xk], lane=xk)
        sq_t, sq_k = f512.next()
        st_, sk = sm.next()
        for hh in range(2):
            act(sq_t[:], xt[:, hh * 512:(hh + 1) * 512], AF.Square, [xk], [sq_k, sk], accum=st_[:, hh:hh + 1])
        tt("dve", st_[:, 2:3], st_[:, 0:1], st_[:, 1:2], ALU.add, [sq_k, sk], [sk])
        act(st_[:, 2:3], st_[:, 2:3], AF.Sqrt, [sk], [sk], scale=1.0 / D, bias=vecs[:, DEPTH * NV_L + 8: DEPTH * NV_L + 9])
        recip(st_[:, 3:4], st_[:, 2:3], [sk], [sk])
        ts("dve", xt[:], xt[:], st_[:, 3:4], None, ALU.mult, ALU.bypass, [xk, sk], [xk])
        for g in range(2):
            pt, pk = pmm.next()
            for c4 in range(4):
                c = g * 4 + c4
                tr(pt[:, c4 * 128:(c4 + 1) * 128], xt[:, c * 128:(c + 1) * 128], ident, [xk, "cst"], [pk])
            cp("act", memhT[:, g * 4:(g + 1) * 4, kt * 128:(kt + 1) * 128],
               pt[:].rearrange("p (c t) -> p c t", c=4), [pk], ["memhT"])

    EPSC = vecs[:, DEPTH * NV_L + 8: DEPTH * NV_L + 9]

    def rmsnorm_to_aT(gcol_off, l, halo=False):
        for s in range(NS):
            sl = slice(s * 512, (s + 1) * 512)
            pt, pk = pmm.next()
            for c in range(8):
                bt, bk = b512.next()
                act(bt[:], hT[:, c, sl], AF.Square, [("hT", c, s)], [bk], eng="act")
                mm(pt[:], onesb, bt[:], c == 0, c == 7, [bk, "cbf"], [pk])
            rt, rk = f512.next()
            act(rt[:], pt[:], AF.Sqrt, [pk, "vecs"], [rk], scale=1.0 / D, bias=EPSC)
            recip(rt[:], rt[:], [rk], [rk])
            for c in range(8):
                stt(aT[:, c, 2 + s * 512: 2 + (s + 1) * 512], hT[:, c, sl], vcol(l, gcol_off + c), rt[:],
                    ALU.mult, ALU.mult, [("hT", c, s), rk, "vecs"], [("aT", c, s)])

    def aTs(c, s):
        return aT[:, c, 2 + s * 512: 2 + (s + 1) * 512]

    def aT_reads(s):
        return [("aT", c, s) for c in range(8)]

    def out_proj(w_d2, tag):
        tiles = [wload(wr, [(lambda t: t[:], rows(w_d2)[:, :, hf * 512:(hf + 1) * 512])]) for hf in range(2)]
        for hf in range(2):
            wt, wk = tiles[hf]
            for m4 in range(4):
                m = hf * 4 + m4
                for s in range(NS):
                    pt, pk = pmm.next()
                    for c in range(8):
                        mm(pt[:], wt[:, c, m4 * 128:(m4 + 1) * 128], oT[:, c, s * 512:(s + 1) * 512], c == 0, c == 7,
                           [wk, ("oT", c, s)], [pk])
                    tt("dve", hT[:, m, s * 512:(s + 1) * 512], hT[:, m, s * 512:(s + 1) * 512], pt[:], ALU.add,
                       [pk, ("hT", m, s)], [("hT", m, s)])

    def head_norm_gate(src_ps_or_sb, src_key, gate_t, gate_k, l, h, s):
        hh_t, hh_k = nrm.next()
        cp("act", hh_t[:], src_ps_or_sb, [src_key], [hh_k])
        bt, bk = b512.next()
        act(bt[:], hh_t[:], AF.Square, [hh_k], [bk])
        pt, pk = pmm.next()
        mm(pt[:], onesb, bt[:], True, True, [bk, "cbf"], [pk])
        rt, rk = nrm.next()
        act(rt[:], pt[:], AF.Sqrt, [pk, "vecs"], [rk], scale=1.0 / 128, bias=EPSC)
        recip(rt[:], rt[:], [rk], [rk])
        stt(hh_t[:], hh_t[:], vcol(l, O_MNG + h), rt[:], ALU.mult, ALU.mult, [hh_k, rk, "vecs"], [hh_k])
        tt("dve", oT[:, h, s * 512:(s + 1) * 512], hh_t[:], gate_t[:], ALU.mult, [hh_k, gate_k], [("oT", h, s)])

    def hgrn2(l):
        j = l // 2
        w_in = hgin_d[j]
        rmsnorm_to_aT(O_GMIX, l)

        def load_head(h):
            return wload(wr, [(lambda t, q=q: t[:, :, q * 128:(q + 1) * 128],
                               rows(w_in)[:, :, q * 1024 + h * 128: q * 1024 + (h + 1) * 128]) for q in range(4)])
        units = [(h, s) for h in range(8) for s in range(NS)]
        wts = {0: load_head(0)}
        U = {}

        def A(i):
            h, s = units[i]
            if s == 0 and h + 1 < 8:
                wts[h + 1] = load_head(h + 1)
            wt, wk = wts[h]
            u = U[i] = dict(h=h, s=s)
            ar = aT_reads(s)
            vt, vk = vtok.next()
            u["vt"], u["vk"] = vt, vk
            for half in range(2):
                pv, pvk = pmm.next()
                for cc in range(4):
                    ch = half * 4 + cc
                    for c in range(8):
                        mm(pv[0:64, cc * 128:(cc + 1) * 128], aT[:, c, 2 + s * 512 + ch * 64: 2 + s * 512 + (ch + 1) * 64],
                           wt[:, c, 256:384], c == 0, c == 7, [wk] + ar, [pvk])
                cp("act", vt[:, half * 4:(half + 1) * 4, 0:128], pv[0:64, :].rearrange("p (c v) -> p c v", c=4), [pvk], [vk])
            ps3 = []
            for q in (0, 1, 3):
                pt, pk = pmm.next()
                for c in range(8):
                    mm(pt[:], wt[:, c, q * 128:(q + 1) * 128], aTs(c, s), c == 0, c == 7, [wk] + ar, [pk])
                ps3.append((pt, pk))
            u["pq"], u["pz"], u["pg"] = ps3

        def B(i):
            u = U[i]
            h = u["h"]
            (pq, pqk), (pz, pzk), (pg, pgk) = u["pq"], u["pz"], u["pg"]
            qs, qsk = f512.next()
            act(qs[:], pq[:], AF.Silu, [pqk], [qsk])
            gs, gsk = gsr.next()
            act(gs[:], pg[:], AF.Silu, [pgk], [gsk])
            u["gs"], u["gsk"] = gs, gsk
            sg, sgk = f512.next()
            act(sg[:], pz[:], AF.Sigmoid, [pzk], [sgk])
            kk, kkk = f512.next()
            act(kk[:], pz[:], AF.Sigmoid, [pzk], [kkk], scale=-1.0)
            act(sg[:], sg[:], AF.Ln, [sgk, ("omlt", l), ("lbt", l)], [sgk], scale=omlt[:, l, h:h + 1], bias=lbt[:, l, h:h + 1])
            bb, bbk = f512.next()
            P.add("dve", lambda e, bb=bb, sg=sg: e.tensor_tensor_scan(out=bb[:], data0=rmask, data1=sg[:], initial=0.0,
                                                                       op0=ALU.mult, op1=ALU.add), [sgk, "cst"], [bbk])
            e1, e1k = f512.next()
            act(e1[:], bb[:], AF.Exp, [bbk], [e1k])
            act(bb[:], bb[:], AF.Exp, [bbk, e1k], [bbk], scale=-1.0)
            eb, ebk = sm8.next()
            cp("dve", eb[:, 0:8], e1[:].rearrange("p (j t) -> p j t", t=64)[:, :, 63], [e1k], [ebk])
            u["eb"], u["ebk"] = eb, ebk
            qt_, qtk = b512.next()
            tt("dve", qt_[:], qs[:], e1[:], ALU.mult, [qsk, e1k], [qtk])
            kt_, ktk = b512.next()
            stt(kt_[:], kk[:], omlt[:, l, h:h + 1], bb[:], ALU.mult, ALU.mult, [kkk, bbk, ("omlt", l)], [ktk])
            kh, khk = b512.next()
            tt("dve", kh[:].rearrange("p (j t) -> p j t", t=64), kt_[:].rearrange("p (j t) -> p j t", t=64),
               eb[:, 0:8].unsqueeze(2).to_broadcast([128, 8, 64]), ALU.mult, [ktk, ebk], [khk])
            u.update(qt=qt_, qtk=qtk, kt=kt_, ktk=ktk, kh=kh, khk=khk)

        def CDE(i):
            u = U[i]
            trt, trk = psm.next()
            trb = trt[:].bitcast(BF16)
            for ch in range(8):
                tr(trb[0:64, ch * 128:(ch + 1) * 128], u["kh"][:, ch * 64:(ch + 1) * 64], identb, [u["khk"], "cbf"], [trk])
            kT_t, kTk = ktok.next()
            cp("act", kT_t[:], trb[0:64, :].rearrange("p (c v) -> p c v", c=8), [trk], [kTk])
            apt, ak = psm.next()
            for ch in range(8):
                cs = slice(ch * 64, (ch + 1) * 64)
                mm(apt[0:64, cs], u["kt"][:, cs], u["qt"][:, cs], True, True, [u["ktk"], u["qtk"]], [ak])
            am, amk = amr.next()
            tt("dve", am[:].rearrange("p (j t) -> p j t", t=64), apt[0:64, :].rearrange("p (j t) -> p j t", t=64),
               maskT.unsqueeze(1).to_broadcast([64, 8, 64]), ALU.mult, [ak, "cst"], [amk])
            u["am"], u["amk"] = am, amk
            ub = []
            for half in range(2):
                ut, uk = psm.next()
                for cc in range(4):
                    ch = half * 4 + cc
                    mm(ut[:, cc * 128:(cc + 1) * 128], kT_t[:, ch, :], u["vt"][:, ch, 0:128], True, True, [kTk, u["vk"]], [uk])
                ub.append((ut, uk))
            u["ub"] = ub

        def FG(i):
            u = U[i]
            h, s = u["h"], u["s"]
            SK = ("Sst", j, h)
            cp("dve", Sall[:, 0, 0:128], Sst[:, j, h, :], [SK], ["Sall"])
            for ch in range(8):
                ut, uk = u["ub"][ch // 4]
                dst = Sall[:, ch + 1, 0:128] if ch < 7 else Sst[:, j, h, :]
                stt(dst, Sall[:, ch, 0:128], u["eb"][:, ch:ch + 1], ut[:, (ch % 4) * 128:(ch % 4 + 1) * 128], ALU.mult, ALU.add,
                    ["Sall", u["ebk"], uk], ["Sall"] if ch < 7 else [SK])
            sb_, sbk = sallb.next()
            cp("act", sb_[:, :, :], Sall[:, 0:8, 0:128], ["Sall"], [sbk])
            u["sb"], u["sbk"] = sb_, sbk

        def H(i):
            u = U[i]
            po, pok = pmm.next()
            for ch in range(8):
                cs = slice(ch * 64, (ch + 1) * 64)
                mm(po[:, cs], u["vt"][:, ch, 0:128], u["am"][:, cs], True, False, [u["vk"], u["amk"]], [pok])
                mm(po[:, cs], u["sb"][:, ch, :], u["qt"][:, cs], False, True, [u["sbk"], u["qtk"]], [pok])
            head_norm_gate(po[:], pok, u["gs"], u["gsk"], l, u["h"], u["s"])
            del U[i]

        A(0)
        B(0)
        for i in range(len(units)):
            CDE(i)
            FG(i)
            if i + 1 < len(units):
                A(i + 1)
                B(i + 1)
            H(i)
        out_proj(hgout_d[j], "hgo")

    def mlstm(l):
        j = l // 2
        w_in = mlin_d[j]
        rmsnorm_to_aT(O_GMIX, l)
        B_, U_, MG, M_, WI, CL, LI, LF = (g8[k] for k in ("B", "u", "Mg", "m", "wi", "cl", "li", "lf"))
        gw, gwk = wload(wr, [(lambda t: t[:, :, 0:16], rows(w_in)[:, :, 3072:3088])])
        cp("dve", B_[:, 0:1], mlcar[:, j, 0:1], ["mlcar"], ["g8B"])
        cp("dve", MG[:, 0:1], mlcar[:, j, 1:2], ["mlcar"], ["g8Mg"])
        for s in range(NS):
            ar = aT_reads(s)
            pi, pik = pmm.next()
            pf, pfk = pmm.next()
            for c in range(8):
                mm(pi[0:8, :], gw[:, c, 0:8], aTs(c, s), c == 0, c == 7, [gwk] + ar, [pik])
            for c in range(8):
                mm(pf[0:8, :], gw[:, c, 8:16], aTs(c, s), c == 0, c == 7, [gwk] + ar, [pfk])
            csl = slice(1 + s * 512, 1 + (s + 1) * 512)
            act(LI[:, csl], pi[0:8, :], AF.Identity, [pik, "vecs"], ["g8li"], bias=vecs[0:8, l * NV_L + O_BI: l * NV_L + O_BI + 1])
            act(LF[:, csl], pf[0:8, :], AF.Sigmoid, [pfk, "vecs"], ["g8lf"], bias=vecs[0:8, l * NV_L + O_BF: l * NV_L + O_BF + 1])
            act(LF[:, csl], LF[:, csl], AF.Ln, ["g8lf"], ["g8lf"])
        P.add("dve", lambda e: e.memset(WI[:, :], 1.0), [], ["g8wi"])
        P.add("dve", lambda e: e.tensor_tensor_scan(out=B_[:, 1:T + 1], data0=WI[:, 1:T + 1], data1=LF[:, 1:T + 1],
                                                     initial=B_[:, 0:1], op0=ALU.mult, op1=ALU.add),
              ["g8lf", "g8wi", "g8B"], ["g8B"])
        cp("dve", mlcar[:, j, 0:1], B_[:, T:T + 1], ["g8B"], ["mlcar"])
        tt("dve", U_[:, 1:T + 1], LI[:, 1:T + 1], B_[:, 1:T + 1], ALU.subtract, ["g8li", "g8B"], ["g8li"])
        P.add("dve", lambda e: e.tensor_tensor_scan(out=MG[:, 1:T + 1], data0=U_[:, 1:T + 1], data1=U_[:, 1:T + 1],
                                                     initial=MG[:, 0:1], op0=ALU.max, op1=ALU.max),
              ["g8li", "g8Mg"], ["g8Mg"])
        tt("dve", M_[:, 1:T + 1], B_[:, 1:T + 1], MG[:, 1:T + 1], ALU.add, ["g8B", "g8Mg"], ["g8B"])
        act(CL[:, 1:T + 1], M_[:, 1:T + 1], AF.Exp, ["g8B"], ["g8B"], scale=-1.0)
        mgp = MG[:, 0:T].rearrange("p (j t) -> p j t", t=64)[:, :, 0:1]
        tt("dve", WI[:, 1:T + 1].rearrange("p (j t) -> p j t", t=64), mgp.to_broadcast([8, NCH, 64]),
           MG[:, 1:T + 1].rearrange("p (j t) -> p j t", t=64), ALU.subtract, ["g8Mg", "g8wi"], ["g8wi"])
        act(WI[:, 1:T + 1], WI[:, 1:T + 1], AF.Exp, ["g8wi"], ["g8wi"])
        cp("dve", mlcar[:, j, 1:2], MG[:, T:T + 1], ["g8Mg"], ["mlcar"])
        for g in range(NCH // 8):
            pbc, pbk = psm.next()
            for cc in range(8):
                ch = g * 8 + cc
                tr(pbc[0:64, cc * 8:(cc + 1) * 8], U_[:, 1 + ch * 64: 1 + (ch + 1) * 64], ident[0:8, 0:8], ["g8li", "cst"], [pbk])
            cp("act", uT[:, g * 8:(g + 1) * 8, :], pbc[0:64, 0:64].rearrange("p (c h) -> p c h", c=8), [pbk], ["uT"])

        def load_head(h):
            return wload(wr, [
                (lambda t: t[:, :, 0:64], rows(w_in)[:, :, h * 64:(h + 1) * 64]),
                (lambda t: t[:, :, 64:128], rows(w_in)[:, :, 512 + h * 64: 512 + (h + 1) * 64]),
                (lambda t: t[:, :, 128:256], rows(w_in)[:, :, 1024 + h * 128: 1024 + (h + 1) * 128]),
                (lambda t: t[:, :, 256:384], rows(w_in)[:, :, 2048 + h * 128: 2048 + (h + 1) * 128]),
            ])
        units = [(h, s) for h in range(8) for s in range(NS)]
        wts = {0: load_head(0)}
        U = {}

        def A(i):
            h, s = units[i]
            if s == 0 and h + 1 < 8:
                wts[h + 1] = load_head(h + 1)
            wt, wk = wts[h]
            u = U[i] = dict(h=h, s=s)
            ar = aT_reads(s)
            vt, vk = vtok.next()
            u["vt"], u["vk"] = vt, vk
            for half in range(2):
                pv, pvk = pmm.next()
                for cc in range(4):
                    ch = half * 4 + cc
                    for c in range(8):
                        mm(pv[0:64, cc * 128:(cc + 1) * 128], aT[:, c, 2 + s * 512 + ch * 64: 2 + s * 512 + (ch + 1) * 64],
                           wt[:, c, 128:256], c == 0, c == 7, [wk] + ar, [pvk])
                cp("act", vt[:, half * 4:(half + 1) * 4, 0:128], pv[0:64, :].rearrange("p (c v) -> p c v", c=4), [pvk], [vk])
            ps3 = []
            for (c0, c1, m) in ((0, 64, 64), (64, 128, 64), (256, 384, 128)):
                pt, pk = pmm.next()
                for c in range(8):
                    mm(pt[0:m, :], wt[:, c, c0:c1], aTs(c, s), c == 0, c == 7, [wk] + ar, [pk])
                ps3.append((pt, pk))
            u["pq"], u["pk"], u["pg"] = ps3
            pkt, pktk = pmm.next()
            for ch in range(8):
                for c in range(8):
                    mm(pkt[0:64, ch * 64:(ch + 1) * 64], aT[:, c, 2 + s * 512 + ch * 64: 2 + s * 512 + (ch + 1) * 64],
                       wt[:, c, 64:128], c == 0, c == 7, [wk] + ar, [pktk])
            u["pkt"], u["pktk"] = pkt, pktk

        def B(i):
            u = U[i]
            h, s = u["h"], u["s"]
            csl = slice(1 + s * 512, 1 + (s + 1) * 512)
            (pq, pqk), (pk_, pkk), (pg, pgk) = u["pq"], u["pk"], u["pg"]
            gs, gsk = gsr.next()
            act(gs[:], pg[:], AF.Sigmoid, [pgk], [gsk])
            u["gs"], u["gsk"] = gs, gsk
            kTs, kTk = b512.next()
            act(kTs[0:64, :], pk_[0:64, :], AF.Copy, [pkk], [kTk], scale=0.125)
            qs, qsk = b512.next()
            cp("act", qs[0:64, :], pq[0:64, :], [pqk], [qsk])
            selh = sel[:, h * 128:(h + 1) * 128]
            pbc, pbk = psm.next()
            mm(pbc[:], selh, MG[:, csl], True, True, ["g8Mg", "cst"], [pbk])
            mgp_t, mgpk = sm8.next()
            cp("act", mgp_t[0:64, 0:8], pbc[0:64, :].rearrange("p (j t) -> p j t", t=64)[:, :, 63], [pbk], [mgpk])
            mgb, mgbk = f512.next()
            tt("dve", mgb[0:64, :].rearrange("p (j t) -> p j t", t=64), negmT.unsqueeze(1).to_broadcast([64, 8, 64]),
               pbc[0:64, :].rearrange("p (j t) -> p j t", t=64), ALU.subtract, [pbk, "cst"], [mgbk])
            uh = uT[:, s * 8:(s + 1) * 8, h]
            tt("dve", mgb[0:64, :].rearrange("p (j t) -> p j t", t=64), mgb[0:64, :].rearrange("p (j t) -> p j t", t=64),
               uh.unsqueeze(2).to_broadcast([64, 8, 64]), ALU.add, [mgbk, "uT"], [mgbk])
            act(mgb[0:64, :], mgb[0:64, :], AF.Exp, [mgbk], [mgbk])
            u["dm"], u["dmk"] = mgb, mgbk
            w_t, w_k = sm8.next()
            tt("dve", w_t[0:64, 0:8], uh, mgp_t[0:64, 0:8], ALU.subtract, ["uT", mgpk], [w_k])
            act(w_t[0:64, 0:8], w_t[0:64, 0:8], AF.Exp, [w_k], [w_k])
            kw, kwk = ktok.next()
            stt(kw[:, :, 0:64], u["pkt"][0:64, :].rearrange("p (j d) -> p j d", d=64), 0.125,
                w_t[0:64, 0:8].unsqueeze(2).to_broadcast([64, 8, 64]), ALU.mult, ALU.mult, [u["pktk"], w_k], [kwk])
            u["kw"], u["kwk"] = kw, kwk
            pbc, pbk = psm.next()
            mm(pbc[:], selh, WI[:, csl], True, True, ["g8wi", "cst"], [pbk])
            dec, deck = sm8.next()
            cp("act", dec[0:64, 0:8], pbc[0:64, :].rearrange("p (j t) -> p j t", t=64)[:, :, 63], [pbk], [deck])
            u["dec"], u["deck"] = dec, deck
            qw, qwk = b512.next()
            tt("dve", qw[0:64, :], pbc[0:64, :], qs[0:64, :], ALU.mult, [qsk, pbk], [qwk])
            pbc, pbk = psm.next()
            mm(pbc[:], selh, CL[:, csl], True, True, ["g8B", "cst"], [pbk])
            clb, clbk = nrm.next()
            cp("act", clb[:], pbc[:], [pbk], [clbk])
            u.update(kTs=kTs, kTk=kTk, qs=qs, qsk=qsk, qw=qw, qwk=qwk, clb=clb, clbk=clbk)

        def CDE(i):
            u = U[i]
            stp, stk = psm.next()
            for ch in range(8):
                cs = slice(ch * 64, (ch + 1) * 64)
                mm(stp[0:64, cs], u["kTs"][0:64, cs], u["qs"][0:64, cs], True, True, [u["kTk"], u["qsk"]], [stk])
            sT, sTk = amr.next()
            tt("dve", sT[:], stp[0:64, :], u["dm"][0:64, :], ALU.mult, [stk, u["dmk"]], [sTk])
            u["sT"], u["sTk"] = sT, sTk
            cb = []
            for g3 in range(3):
                ct, ck = psm.next()
                for cc in range(3):
                    ch = g3 * 3 + cc
                    if ch < 8:
                        mm(ct[0:64, cc * 160: cc * 160 + 129], u["kw"][:, ch, 0:64], u["vt"][:, ch, 0:129], True, True,
                           [u["kwk"], u["vk"]], [ck])
                cb.append((ct, ck))
            u["cb"] = cb

        def FG(i):
            u = U[i]
            h = u["h"]
            CK = ("Cst", j, h)
            cp("dve", Sall[0:64, 0, :], Cst[:, j, h, :], [CK], ["Sall"])
            for ch in range(8):
                ct, ck = u["cb"][ch // 3]
                dst = Sall[0:64, ch + 1, :] if ch < 7 else Cst[:, j, h, :]
                stt(dst, Sall[0:64, ch, :], u["dec"][0:64, ch:ch + 1], ct[0:64, (ch % 3) * 160:(ch % 3) * 160 + 129], ALU.mult, ALU.add,
                    ["Sall", u["deck"], ck], ["Sall"] if ch < 7 else [CK])
            sb_, sbk = sallb.next()
            cp("act", sb_[0:64, :, :], Sall[0:64, 0:8, 0:128], ["Sall"], [sbk])
            nb, nbk = sallb.next()
            tt("dve", nb[0:64, :, :], onesb[0:64, :].unsqueeze(1).to_broadcast([64, 8, 128]),
               Sall[0:64, 0:8, 128:129].to_broadcast([64, 8, 128]), ALU.mult, ["Sall", "cbf"], [nbk])
            u.update(sb=sb_, sbk=sbk, nb=nb, nbk=nbk)

        def H(i):
            u = U[i]
            pnum, pnk = pmm.next()
            pden, pdk = pmm.next()
            for ch in range(8):
                cs = slice(ch * 64, (ch + 1) * 64)
                mm(pnum[:, cs], u["vt"][:, ch, 0:128], u["sT"][:, cs], True, False, [u["vk"], u["sTk"]], [pnk])
                mm(pnum[:, cs], u["sb"][0:64, ch, :], u["qw"][0:64, cs], False, True, [u["sbk"], u["qwk"]], [pnk])
            for ch in range(8):
                cs = slice(ch * 64, (ch + 1) * 64)
                mm(pden[:, cs], onesb[0:64, :], u["sT"][:, cs], True, False, ["cbf", u["sTk"]], [pdk])
                mm(pden[:, cs], u["nb"][0:64, ch, :], u["qw"][0:64, cs], False, True, [u["nbk"], u["qwk"]], [pdk])
            dd, ddk = f512.next()
            act(dd[:], pden[:], AF.Abs, [pdk], [ddk])
            tt("dve", dd[:], dd[:], u["clb"][:], ALU.max, [ddk, u["clbk"]], [ddk])
            recip(dd[:], dd[:], [ddk], [ddk])
            hh, hhk = f512.next()
            tt("dve", hh[:], pnum[:], dd[:], ALU.mult, [pnk, ddk], [hhk])
            head_norm_gate(hh[:], hhk, u["gs"], u["gsk"], l, u["h"], u["s"])
            del U[i]

        A(0)
        B(0)
        for i in range(len(units)):
            CDE(i)
            FG(i)
            if i + 1 < len(units):
                A(i + 1)
                B(i + 1)
            H(i)
        out_proj(mlout_d[j], "mlo")

    def xattn(l):
        rmsnorm_to_aT(O_GXA, l)
        for c in range(8):
            ts("dve", memnT[:, c, :], memhT[:, c, :], vcol(l, O_GMEM + c), None, ALU.mult, ALU.bypass,
               ["memhT", "vecs"], ["memnT"])
        for hf in range(2):
            wt, wk = wload(wr, [(lambda t: t[:], rows(wkv_d[l])[:, :, hf * 512:(hf + 1) * 512])])
            for m4 in range(4):
                pt, pk = pmm.next()
                for c in range(8):
                    mm(pt[:, 0:NMEM], wt[:, c, m4 * 128:(m4 + 1) * 128], memnT[:, c, :], c == 0, c == 7, [wk, "memnT"], [pk])
                cp("act", KT[:, hf * 4 + m4, :], pt[:, 0:NMEM], [pk], ["KT"])
        for hf in range(2):
            wt, wk = wload(wr, [(lambda t: t[:], rows(wkv_d[l])[:, :, D + hf * 512: D + (hf + 1) * 512])])
            for kt in range(2):
                pt, pk = pmm.next()
                for c in range(8):
                    mm(pt[:], memnT[:, c, kt * 128:(kt + 1) * 128], wt[:, c, :], c == 0, c == 7, [wk, "memnT"], [pk])
                cp("act", Vt[:, kt, hf * 512:(hf + 1) * 512], pt[:], [pk], ["Vt"])
        for hp in range(2):
            wt, wk = wload(wr, [(lambda t: t[:], rows(wq_d[l])[:, :, hp * 512:(hp + 1) * 512])])
            for hh_ in range(2):
                hd = hp * 2 + hh_
                for s in range(NS):
                    ar = aT_reads(s)
                    q0, q0k = qx.next()
                    q1, q1k = qx.next()
                    qts = ((q0, q0k), (q1, q1k))
                    for dc in range(2):
                        pt, pk = pmm.next()
                        for c in range(8):
                            mm(pt[:], wt[:, c, hh_ * 256 + dc * 128: hh_ * 256 + (dc + 1) * 128], aTs(c, s), c == 0, c == 7,
                               [wk] + ar, [pk])
                        act(qts[dc][0][:], pt[:], AF.Copy, [pk], [qts[dc][1]], scale=1.0 / 16.0)
                    po0, po0k = pmm.next()
                    po1, po1k = pmm.next()
                    pos = ((po0, po0k), (po1, po1k))
                    for tt_ in range(4):
                        tsl = slice(tt_ * 128, (tt_ + 1) * 128)
                        sct, sck = psm.next()
                        sc = sct[:, 0:256]
                        scks = [sck]
                        for dc in range(2):
                            mm(sc, qts[dc][0][:, tsl], KT[:, hd * 2 + dc, :], dc == 0, dc == 1, [qts[dc][1], "KT"], scks)
                        st_, sk = sm.next()
                        P.add("dve", lambda e, st_=st_, sc=sc: e.tensor_reduce(out=st_[:, 0:1], in_=sc, axis=AX.X, op=ALU.max, negate=True),
                              scks, [sk])
                        ee, eek = f512.next()
                        act(ee[:, 0:256], sc, AF.Exp, scks + [sk], [eek, sk], bias=st_[:, 0:1], accum=st_[:, 1:2])
                        recip(st_[:, 2:3], st_[:, 1:2], [eek, sk], [sk])
                        pp, ppk = b512.next()
                        ts("dve", pp[:, 0:256], ee[:, 0:256], st_[:, 2:3], None, ALU.mult, ALU.bypass, [eek, sk], [ppk])
                        trt, trk = psm.next()
                        trb = trt[:].bitcast(BF16)
                        for kt in range(2):
                            tr(trb[:, kt * 128:(kt + 1) * 128], pp[:, kt * 128:(kt + 1) * 128], identb, [ppk, "cbf"], [trk])
                        pT, pTk = b512.next()
                        cp("act", pT[:, 0:256], trb[:, 0:256], [trk], [pTk])
                        for dc in range(2):
                            for kt in range(2):
                                mm(pos[dc][0][:, tsl], Vt[:, kt, hd * 256 + dc * 128: hd * 256 + (dc + 1) * 128],
                                   pT[:, kt * 128:(kt + 1) * 128], kt == 0, kt == 1, ["Vt", pTk], [pos[dc][1]])
                    for dc in range(2):
                        cp("act", oT[:, hd * 2 + dc, s * 512:(s + 1) * 512], pos[dc][0][:], [pos[dc][1]], [("oT", hd * 2 + dc, s)])
        out_proj(wo_d[l], "xo")

    nsub = -(-T // 510)
    bnds = [round(i * T / nsub) for i in range(nsub + 1)]

    def ffn(l, wave):
        rmsnorm_to_aT(O_GFFN, l)
        cp("dve", aT[:, :, 0:2], ffhalo[:, l, :, :], ["ffhalo"], [("aTh",)])
        cp("dve", ffhalo[:, l, :, :], aT[:, :, T:T + 2], [("aT", c, NS - 1) for c in range(8)], ["ffhalo"])

        def load_g(g):
            return wload(wr, [(lambda t: t[:, :, 0:256], rows(wup_d[l])[:, :, g * 256:(g + 1) * 256]),
                              (lambda t: t[:, :, 256:512], rows(wup_d[l])[:, :, DFF + g * 256: DFF + (g + 1) * 256])])
        nxt = load_g(0)
        for g in range(NFF // 2):
            wt, wk = nxt
            if g + 1 < NFF // 2:
                nxt = load_g(g + 1)
            for jj in range(2):
                jf = g * 2 + jj
                for si in range(nsub):
                    a, b = bnds[si], bnds[si + 1]
                    n = b - a
                    rd = [wk, ("aTh",)] + [("aT", c, s) for c in range(8) for s in range(NS)]
                    pgt, pgk = pmm.next()
                    pvt, pvk = pmm.next()
                    for c in range(8):
                        mm(pgt[:, 0:n + 2], wt[:, c, jj * 128:(jj + 1) * 128], aT[:, c, a:b + 2], c == 0, c == 7, rd, [pgk])
                    for c in range(8):
                        mm(pvt[:, 0:n + 2], wt[:, c, 256 + jj * 128: 256 + (jj + 1) * 128], aT[:, c, a:b + 2], c == 0, c == 7, rd, [pvk])
                    outs = []
                    for (pt, pk, col) in ((pgt, pgk, jf), (pvt, pvk, NFF + jf)):
                        y, yk = f512.next()
                        act(y[:, 0:n], pt[:, 2:n + 2], AF.Identity, [pk, "vecs"], [yk],
                            scale=vcol(l, O_CW2 + col), bias=vcol(l, O_CB + col))
                        stt(y[:, 0:n], pt[:, 1:n + 1], vcol(l, O_CW1 + col), y[:, 0:n], ALU.mult, ALU.add, [pk, yk, "vecs"], [yk])
                        stt(y[:, 0:n], pt[:, 0:n], vcol(l, O_CW0 + col), y[:, 0:n], ALU.mult, ALU.add, [pk, yk, "vecs"], [yk])
                        outs.append((y, yk))
                    (yg, ygk), (yv, yvk) = outs
                    act(yg[:, 0:n], yg[:, 0:n], AF.Silu, [ygk], [ygk])
                    tt("dve", prod[:, jf, a:b], yg[:, 0:n], yv[:, 0:n], ALU.mult, [ygk, yvk], [("prod", jf)])
        def load_d(mp):
            return wload(wdr, [(lambda t: t[:], wdn_d[l].rearrange("(j p) n -> p j n", p=128)[:, :, mp * 128:(mp + 1) * 128])])
        nxt = load_d(0)
        for mp in range(8):
            wt, wk = nxt
            if mp + 1 < 8:
                nxt = load_d(mp + 1)
            for m2 in range(1):
                m = mp
                for s in range(NS):
                    pt, pk = pmm.next()
                    for jf in range(NFF):
                        mm(pt[:], wt[:, jf, m2 * 128:(m2 + 1) * 128], prod[:, jf, s * 512:(s + 1) * 512], jf == 0, jf == NFF - 1,
                           [wk, ("prod", jf)], [pk])
                    tt("dve", hT[:, m, s * 512:(s + 1) * 512], hT[:, m, s * 512:(s + 1) * 512], pt[:], ALU.add,
                       [pk, ("hT", m, s)], [("hT", m, s)])

    olanes = []
    for w in range(NW):
        for t4 in range(T // 128):
            s = t4 // 4
            xt, xk = xin.next()
            r0 = w * T + t4 * 128
            P.dma("sp", xt[:], x_d[r0:r0 + 128, :], writes=[xk], lane=xk)
            for g in range(2):
                pt, pk = pmm.next()
                for c4 in range(4):
                    c = g * 4 + c4
                    tr(pt[:, c4 * 128:(c4 + 1) * 128], xt[:, c * 128:(c + 1) * 128], ident, [xk, "cst"], [pk])
                cp("act", hT[:, g * 4:(g + 1) * 4, t4 * 128:(t4 + 1) * 128], pt[:].rearrange("p (c t) -> p c t", c=4), [pk],
                   [("hT", c, s) for c in range(g * 4, g * 4 + 4)])
        for l in layers:
            if "m" in phases:
                if l % 2 == 0:
                    hgrn2(l)
                else:
                    mlstm(l)
            if "x" in phases:
                xattn(l)
            if "f" in phases:
                ffn(l, w)
        for s in range(NS):
            sl = slice(s * 512, (s + 1) * 512)
            if final:
                pt, pk = pmm.next()
                for c in range(8):
                    bt, bk = b512.next()
                    act(bt[:], hT[:, c, sl], AF.Square, [("hT", c, s)], [bk])
                    mm(pt[:], onesb, bt[:], c == 0, c == 7, [bk, "cbf"], [pk])
                rt, rk = nrm.next()
                act(rt[:], pt[:], AF.Sqrt, [pk, "vecs"], [rk], scale=1.0 / D, bias=EPSC)
                recip(rt[:], rt[:], [rk], [rk])
            for t4 in range(4):
                tsl = slice(s * 512 + t4 * 128, s * 512 + (t4 + 1) * 128)
                xt, xk = xin.next()
                for g in range(2):
                    pt2, pk2 = pmm.next()
                    for c4 in range(4):
                        c = g * 4 + c4
                        if final:
                            yt, yk = f512.next()
                            stt(yt[:, 0:128], hT[:, c, tsl], vecs[:, DEPTH * NV_L + c: DEPTH * NV_L + c + 1], rt[:, t4 * 128:(t4 + 1) * 128], ALU.mult, ALU.mult,
                                [("hT", c, s), rk, "vecs"], [yk])
                            tr(pt2[:, c4 * 128:(c4 + 1) * 128], yt[:, 0:128], ident, [yk, "cst"], [pk2])
                        else:
                            tr(pt2[:, c4 * 128:(c4 + 1) * 128], hT[:, c, tsl], ident, [("hT", c, s), "cst"], [pk2])
                    cp("act", xt[:, g * 512:(g + 1) * 512], pt2[:], [pk2], [xk])
                r0 = w * T + s * 512 + t4 * 128
                ln = ("o", xk)
                P.dma("sp", out_d[r0:r0 + 128, :], xt[:], reads=[xk], writes=[("outrow", r0)], lane=ln)
                if ln not in olanes:
                    olanes.append(ln)
    P.finish(final_lanes=olanes)
    return nc, P.stats


def _consts():
    cst = np.zeros((128, 128 + 64 + 64 + 512 + 1024), np.float32)
    cst[:, 0:128] = np.eye(128, dtype=np.float32)
    s = np.arange(64)[:, None]
    t = np.arange(64)[None, :]
    cst[0:64, 128:192] = (s <= t).astype(np.float32)
    cst[0:64, 192:256] = np.where(s <= t, 0.0, -1e30).astype(np.float32)
    rm = np.ones(512, np.float32)
    rm[::64] = 0.0
    cst[:, 256:768] = rm[None, :]
    for h in range(8):
        cst[h, 768 + h * 128: 768 + (h + 1) * 128] = 1.0
    cbf = np.zeros((128, 256), np.float32)
    cbf[:, 0:128] = np.eye(128, dtype=np.float32)
    cbf[:, 128:256] = 1.0
    return cst, cbf.astype(ml_dtypes.bfloat16)


def _cols(v, n):
    return np.ascontiguousarray(np.asarray(v, np.float32).reshape(n, 128).T)


def _pack_vecs(inp):
    vec = np.zeros((128, DEPTH * NV_L + 16), np.float32)
    for l in range(DEPTH):
        b = l * NV_L
        j = l // 2
        vec[:, b + 0:b + 8] = _cols(inp["norm_mix_g"][l], 8)
        vec[:, b + 8:b + 16] = _cols(inp["norm_xa_g"][l], 8)
        vec[:, b + 16:b + 24] = _cols(inp["norm_mem_g"][l], 8)
        vec[:, b + 24:b + 32] = _cols(inp["norm_ffn_g"][l], 8)
        vec[:, b + 32:b + 76] = _cols(inp["ffn_conv_w"][l][0], 44)
        vec[:, b + 76:b + 120] = _cols(inp["ffn_conv_w"][l][1], 44)
        vec[:, b + 120:b + 164] = _cols(inp["ffn_conv_w"][l][2], 44)
        vec[:, b + 164:b + 208] = _cols(inp["ffn_conv_b"][l], 44)
        vec[:, b + 208:b + 216] = _cols((inp["hg_norm_g"] if l % 2 == 0 else inp["ml_norm_g"])[j], 8)
        vec[:, b + 216:b + 224] = _cols(inp["hg_lb_logits"][l], 8)
        if l % 2 == 1:
            vec[0:8, b + 224] = np.asarray(inp["ml_b_gate"][j][0:8], np.float32)
            vec[0:8, b + 225] = np.asarray(inp["ml_b_gate"][j][8:16], np.float32)
    vec[:, DEPTH * NV_L: DEPTH * NV_L + 8] = _cols(inp["final_g"], 8)
    vec[:, DEPTH * NV_L + 8] = EPS
    return vec


_CACHE = {}
MODE = "fused"
WAVE_T = 512

_WNAMES = ("hg_w_in", "hg_w_out", "ml_w_in", "ml_w_out", "xa_wq", "xa_wkv", "xa_wo", "ffn_w_up", "ffn_w_down")


def _get_nc(**kw):
    key = tuple(sorted(kw.items()))
    if key not in _CACHE:
        _CACHE[key] = build(**kw)[0]
    return _CACHE[key]


def _launch(nc, xs, inp, vec, cst, cbf, n):
    shared = {k: np.ascontiguousarray(inp[k], dtype=np.float32) for k in _WNAMES}
    in_maps = []
    for b in range(n):
        m = dict(shared)
        m["x"] = np.ascontiguousarray(xs[b], dtype=np.float32)
        m["mem"] = np.ascontiguousarray(inp["mem"][b], dtype=np.float32)
        m["vecs"] = vec
        m["cst"] = cst
        m["cbf"] = cbf
        in_maps.append(m)
    res = run_bass_kernel_spmd(nc, in_maps, core_ids=list(range(n)))
    return [r["out"] for r in res.results]


def kernel(**inputs):
    inp = {k: np.asarray(v) for k, v in inputs.items()}
    vec = _pack_vecs(inp)
    cst, cbf = _consts()
    x = inp["x"]
    n = x.shape[0]
    S = x.shape[1]
    if MODE == "fused":
        nc = _get_nc(S=S, T=WAVE_T, layers=(0, 1, 2, 3), final=True)
        outs = _launch(nc, [x[b] for b in range(n)], inp, vec, cst, cbf, n)
    else:
        hs = [x[b] for b in range(n)]
        for l in range(DEPTH):
            nc = _get_nc(S=S, T=WAVE_T, layers=(l,), final=(l == DEPTH - 1))
            hs = _launch(nc, hs, inp, vec, cst, cbf, n)
        outs = hs
    return np.stack(outs, axis=0).astype(np.float32)
```

```python
import numpy as np
import ml_dtypes
import concourse.bass as bass
import concourse.mybir as mybir
from concourse.bass_utils import run_bass_kernel_spmd
from contextlib import ExitStack

F32 = mybir.dt.float32
BF16 = mybir.dt.bfloat16
AF = mybir.ActivationFunctionType
ALU = mybir.AluOpType
AX = mybir.AxisListType

ENGS = ("pe", "act", "dve", "pool", "sp")

D = 1024
SEQ = 4096
NMEM = 256
DEPTH = 4
DFF = 2816
NFF = DFF // 128
EPS = 1e-6
HG_IN = 4096
ML_IN = 3088
NV_L = 232


class Prog:
    def __init__(self, nc):
        self.nc = nc
        self.ops = []
        self.stack = ExitStack()
        self.nt = 0
        self.psum_keys = set()

    def sb(self, shape, dtype, name=None):
        self.nt += 1
        return self.stack.enter_context(self.nc.sbuf_tensor("sb_" + (name or f"t{self.nt}"), list(shape), dtype))

    def ps(self, shape, dtype, name=None):
        self.nt += 1
        return self.stack.enter_context(self.nc.psum_tensor("ps_" + (name or f"p{self.nt}"), list(shape), dtype))

    def add(self, eng, fn, reads=(), writes=(), lane=None):
        self.ops.append((eng, fn, tuple(reads), tuple(writes), lane))

    def dma(self, q, out, in_, reads=(), writes=(), lane=None):
        assert lane is not None
        self.ops.append((q, (lambda e, o=out, i=in_: e.dma_start(out=o, in_=i)), tuple(reads), tuple(writes), lane))

    def finish(self, final_lanes=()):
        nc = self.nc
        ops = self.ops
        n = len(ops)
        last_w = {}
        readers = {}
        know = {e: {} for e in ENGS}
        vc = [None] * n
        waits = [None] * n
        signaled = [False] * n
        lane_cnt = {}
        dma_cnt = [0] * n
        for i, (eng, fn, reads, writes, lane) in enumerate(ops):
            deps = set()
            raw = set()
            for r in reads:
                j = last_w.get(r)
                if j is not None:
                    deps.add(j)
                    raw.add(j)
                if r in self.psum_keys:
                    for j in readers.get(r, ()):
                        deps.add(j)
            for w in writes:
                j = last_w.get(w)
                if j is not None:
                    deps.add(j)
                for j in readers.get(w, ()):
                    deps.add(j)
            kn = know[eng]
            wl = []
            for j in sorted(deps, reverse=True):
                je, _, _, _, jl = ops[j]
                if jl is not None:
                    key = ("L", jl)
                    val = dma_cnt[j]
                else:
                    if je == eng and lane is None and (eng == "pe" or j not in raw):
                        continue
                    key = je
                    val = j
                if kn.get(key, -1) >= val:
                    continue
                wl.append(j)
                if jl is None:
                    signaled[j] = True
                for k2, v2 in vc[j].items():
                    if kn.get(k2, -1) < v2:
                        kn[k2] = v2
            waits[i] = wl
            v = dict(kn)
            if lane is not None:
                c = lane_cnt.get(lane, 0) + 1
                lane_cnt[lane] = c
                dma_cnt[i] = c
                v[("L", lane)] = c
            else:
                v[eng] = i
            vc[i] = v
            for r in reads:
                readers.setdefault(r, []).append(i)
            for w in writes:
                last_w[w] = i
                readers[w] = []
        sig = [0] * n
        cnt = {e: 0 for e in ENGS}
        for i, (eng, fn, reads, writes, lane) in enumerate(ops):
            if lane is None and signaled[i]:
                cnt[eng] += 1
                sig[i] = cnt[eng]
        self.stats = dict(n_ops=n, signals=dict(cnt), lanes=len(lane_cnt),
                          n_waits=sum(len(w) for w in waits),
                          per_eng={e: sum(1 for o in ops if o[0] == e) for e in ENGS})
        st = self.stack
        esem = {e: st.enter_context(nc.semaphore(f"s_{e}")) for e in ENGS}
        lsem = {l: st.enter_context(nc.semaphore(f"l_{k}")) for k, l in enumerate(lane_cnt)}
        per_eng = {e: [] for e in ENGS}
        for i, op in enumerate(ops):
            per_eng[op[0]].append(i)

        def emit(eng_name, e):
            for i in per_eng[eng_name]:
                _, fn, _, _, lane = ops[i]
                for j in waits[i]:
                    je, _, _, _, jl = ops[j]
                    if jl is not None:
                        e.wait_ge(lsem[jl], 16 * dma_cnt[j])
                    else:
                        e.wait_ge(esem[je], sig[j])
                ins = fn(e)
                if lane is not None:
                    ins.then_inc(lsem[lane], 16)
                elif signaled[i]:
                    ins.then_inc(esem[eng_name], 1)
            if eng_name == "sp":
                for l in final_lanes:
                    e.wait_ge(lsem[l], 16 * lane_cnt[l])

        with nc.Block() as block:
            @block.tensor
            def _(e):
                emit("pe", e)

            @block.scalar
            def _(e):
                emit("act", e)

            @block.vector
            def _(e):
                emit("dve", e)

            @block.gpsimd
            def _(e):
                emit("pool", e)

            @block.sync
            def _(e):
                emit("sp", e)
        st.close()


class Ring:
    def __init__(self, P, name, n, shape, dtype, psum=False, tiles=None):
        self.name = name
        self.n = n
        self.i = 0
        self.tiles = tiles if tiles is not None else [(P.ps if psum else P.sb)(shape, dtype, f"{name}{k}") for k in range(n)]
        if psum:
            for k in range(n):
                P.psum_keys.add((name, k))

    def next(self):
        k = self.i % self.n
        self.i += 1
        return self.tiles[k], (self.name, k)


def build(S=SEQ, T=1024, layers=(0, 1, 2, 3), final=True, phases="mxf"):
    nc = bass.Bass("TRN2", target_bir_lowering=False)
    P = Prog(nc)
    NW = S // T
    NS = T // 512
    NCH = T // 64
    L0 = layers[0]

    def din(name, shape, dt=F32):
        return nc.dram_tensor(name, list(shape), dt, kind="ExternalInput").ap()

    x_d = din("x", [S, D])
    mem_d = din("mem", [NMEM, D])
    vec_d = din("vecs", [128, DEPTH * NV_L + 16])
    cst_d = din("cst", [128, 128 + 64 + 64 + 512 + 1024])
    cbf_d = din("cbf", [128, 256], BF16)
    hgin_d = din("hg_w_in", [2, D, HG_IN])
    hgout_d = din("hg_w_out", [2, D, D])
    mlin_d = din("ml_w_in", [2, D, ML_IN])
    mlout_d = din("ml_w_out", [2, D, D])
    wq_d = din("xa_wq", [DEPTH, D, D])
    wkv_d = din("xa_wkv", [DEPTH, D, 2 * D])
    wo_d = din("xa_wo", [DEPTH, D, D])
    wup_d = din("ffn_w_up", [DEPTH, D, 2 * DFF])
    wdn_d = din("ffn_w_down", [DEPTH, DFF, D])
    out_d = nc.dram_tensor("out", [S, D], F32, kind="ExternalOutput").ap()

    hT = P.sb([128, 8, T], F32, "hT")
    aT = P.sb([128, 8, T + 2], BF16, "aT")
    prod = P.sb([128, NFF, T], BF16, "prod")
    oT = prod[:, 0:8, :]
    vecs = P.sb([128, DEPTH * NV_L + 16], F32, "vecs")
    cst = P.sb([128, 128 + 64 + 64 + 512 + 1024], F32, "cst")
    cbf = P.sb([128, 256], BF16, "cbf")
    lbt = P.sb([128, DEPTH, 8], F32, "lbt")
    omlt = P.sb([128, DEPTH, 8], F32, "omlt")
    memhT = P.sb([128, 8, NMEM], BF16, "memhT")
    memnT = P.sb([128, 8, NMEM], BF16, "memnT")
    KT = P.sb([128, 8, NMEM], BF16, "KT")
    Vt = P.sb([128, 2, D], BF16, "Vt")
    ffhalo = P.sb([128, DEPTH, 8, 2], BF16, "ffhalo")
    Sst = P.sb([128, 2, 8, 128], F32, "Sst")
    Cst = P.sb([64, 2, 8, 129], F32, "Cst")
    mlcar = P.sb([8, 2, 2], F32, "mlcar")

    ident = cst[:, 0:128]
    maskT = cst[0:64, 128:192]
    negmT = cst[0:64, 192:256]
    rmask = cst[:, 256:768]
    sel = cst[0:8, 768:1792]
    identb = cbf[:, 0:128]
    onesb = cbf[:, 128:256]

    def vcol(l, off, n=1):
        return vecs[:, l * NV_L + off: l * NV_L + off + n]
    O_GMIX, O_GXA, O_GMEM, O_GFFN, O_CW0, O_CW1, O_CW2, O_CB, O_MNG, O_LB, O_BI, O_BF = 0, 8, 16, 24, 32, 76, 120, 164, 208, 216, 224, 225

    wr = Ring(P, "w", 3, [128, 8, 512], BF16)
    wdr = Ring(P, "wd", 2, [128, NFF, 128], BF16)
    xin = Ring(P, "xin", 2, [128, D], F32)
    pbig = [P.ps([128, 1024], F32, f"pbig{k}") for k in range(4)]
    pmm = Ring(P, "pmm", 4, None, None, psum=True, tiles=[pbig[k // 2][:, (k % 2) * 512:(k % 2 + 1) * 512] for k in range(4)])
    psm = Ring(P, "psm", 4, None, None, psum=True, tiles=[pbig[2 + k // 2][:, (k % 2) * 512:(k % 2 + 1) * 512] for k in range(4)])
    pdb_keys = [[("pmm", 0), ("pmm", 1)], [("pmm", 2), ("pmm", 3)], [("psm", 0), ("psm", 1)], [("psm", 2), ("psm", 3)]]
    pdb_i = [0]

    def pdb_next():
        k = pdb_i[0] % 4
        pdb_i[0] += 1
        return pbig[k], pdb_keys[k]
    f512 = Ring(P, "f512", 6, [128, 512], F32)
    fu = Ring(P, "fu", 4, [128, 516], F32)
    nrm = Ring(P, "nrm", 3, [128, 512], F32)
    gsr = Ring(P, "gsr", 3, [128, 512], BF16)
    sm8 = Ring(P, "sm8", 8, [128, 8], F32)
    amr = Ring(P, "amr", 2, [64, 512], BF16)
    sallb = Ring(P, "sallb", 2, [128, 8, 128], BF16)
    clr = Ring(P, "clr", 2, [128, 512], F32)
    Sall = P.sb([128, 9, 129], F32, "Sall")
    b512 = Ring(P, "b512", 8, [128, 512], BF16)
    sm = Ring(P, "sm", 8, [128, 4], F32)
    vtok = Ring(P, "vtok", 2, [64, 8, 130], BF16)
    ktok = Ring(P, "ktok", 2, [64, 8, 128], BF16)
    qx = Ring(P, "qx", 4, [128, 512], BF16)
    _g8 = {k: P.sb([8, T + 1], F32, f"g8{k}") for k in ("li", "lf", "B", "Mg", "wi")}
    g8 = dict(li=_g8["li"], u=_g8["li"], lf=_g8["lf"], B=_g8["B"], m=_g8["B"], cl=_g8["B"], Mg=_g8["Mg"], wi=_g8["wi"])
    uT = P.sb([64, NCH, 8], F32, "uT")

    cnt = [0]

    def uid():
        cnt[0] += 1
        return cnt[0]

    def mm(out, lhsT, rhs, start, stop, reads, writes):
        P.add("pe", lambda e: e.matmul(out, lhsT=lhsT, rhs=rhs, start=start, stop=stop), reads, writes)

    def tr(out, in_, idn, reads, writes):
        P.add("pe", lambda e: e.transpose(out, in_, idn), reads, writes)

    def act(out, in_, func, reads, writes, scale=1.0, bias=None, accum=None, eng="act"):
        kw = {}
        if bias is not None:
            kw["bias"] = bias
        if accum is not None:
            kw["accum_out"] = accum
        P.add(eng, lambda e: e.activation(out=out, in_=in_, func=func, scale=scale, **kw), reads, writes)

    def tt(eng, out, in0, in1, op, reads, writes):
        P.add(eng, lambda e: e.tensor_tensor(out=out, in0=in0, in1=in1, op=op), reads, writes)

    def ts(eng, out, in0, s1, s2, op0, op1, reads, writes):
        P.add(eng, lambda e: e.tensor_scalar(out=out, in0=in0, scalar1=s1, scalar2=s2, op0=op0, op1=op1), reads, writes)

    def stt(out, in0, scalar, in1, op0, op1, reads, writes):
        P.add("dve", lambda e: e.scalar_tensor_tensor(out=out, in0=in0, scalar=scalar, in1=in1, op0=op0, op1=op1), reads, writes)

    def cp(eng, out, in_, reads, writes):
        if eng == "act":
            act(out, in_, AF.Copy, reads, writes)
        else:
            P.add(eng, lambda e: e.tensor_copy(out=out, in_=in_), reads, writes)

    def recip(out, in_, reads, writes):
        P.add("dve", lambda e: e.reciprocal(out=out, in_=in_), reads, writes)

    def wload(ring, parts):
        t, k = ring.next()
        for dst_fn, src in parts:
            P.dma("pool", dst_fn(t), src, writes=[k], lane=k)
        return t, k

    def rows(w2d):
        return w2d.rearrange("(c p) n -> p c n", p=128)

    P.dma("sp", vecs[:], vec_d, writes=["vecs"], lane="c0")
    P.dma("sp", cst[:], cst_d, writes=["cst"], lane="c1")
    P.dma("sp", cbf[:], cbf_d, writes=["cbf"], lane="c2")
    CR = ["vecs", "cst", "cbf"]

    ex = P.sb([128, DEPTH, 8], F32, "ex")
    mxl = P.sb([128, 8], F32, "mxl")
    sml = P.sb([128, 8], F32, "sml")
    lg = [vcol(l, O_LB, 8) for l in range(DEPTH)]
    tt("dve", mxl[:], lg[0], lg[1], ALU.max, CR, ["mxl"])
    tt("dve", mxl[:], mxl[:], lg[2], ALU.max, CR + ["mxl"], ["mxl"])
    tt("dve", mxl[:], mxl[:], lg[3], ALU.max, CR + ["mxl"], ["mxl"])
    for l in range(DEPTH):
        tt("dve", ex[:, l, :], lg[l], mxl[:], ALU.subtract, CR + ["mxl"], [("ex", l)])
        act(ex[:, l, :], ex[:, l, :], AF.Exp, [("ex", l)], [("ex", l)])
    tt("dve", sml[:], ex[:, 0, :], ex[:, 1, :], ALU.add, [("ex", 0), ("ex", 1)], ["sml"])
    tt("dve", sml[:], sml[:], ex[:, 2, :], ALU.add, ["sml", ("ex", 2)], ["sml"])
    tt("dve", sml[:], sml[:], ex[:, 3, :], ALU.add, ["sml", ("ex", 3)], ["sml"])
    recip(sml[:], sml[:], ["sml"], ["sml"])
    P.add("dve", lambda e: e.memset(lbt[:, 0, :], 0.0), [], [("lbt", 0)])
    for l in range(1, DEPTH):
        if l == 1:
            cp("dve", lbt[:, 1, :], ex[:, 1, :], [("ex", 1)], [("lbt", 1)])
        else:
            tt("dve", lbt[:, l, :], lbt[:, l - 1, :], ex[:, l, :], ALU.add, [("lbt", l - 1), ("ex", l)], [("lbt", l)])
    for l in range(1, DEPTH):
        pass
    for l in range(DEPTH - 1, 0, -1):
        tt("dve", lbt[:, l, :], lbt[:, l, :], sml[:], ALU.mult, [("lbt", l), "sml"], [("lbt", l)])
    for l in range(DEPTH):
        ts("dve", omlt[:, l, :], lbt[:, l, :], -1.0, 1.0, ALU.mult, ALU.add, [("lbt", l)], [("omlt", l)])

    P.add("pool", lambda e: e.memset(Sst[:], 0.0), [], ["Sst"])
    P.add("pool", lambda e: e.memset(Cst[:], 0.0), [], ["Cst"])
    P.add("pool", lambda e: e.memset(mlcar[:], 0.0), [], ["mlcar"])
    P.add("pool", lambda e: e.memset(ffhalo[:], 0.0), [], ["ffhalo"])
    for k_, r in enumerate(vtok.tiles):
        P.add("pool", lambda e, r=r: e.memset(r[:, :, 128:130], 1.0), [], [("vtok", k_)])

    for kt in range(2):
        xt, xk = xin.next()
        P.dma("sp", xt[:], mem_d[kt * 128:(kt + 1) * 128, :], writes=[xk], lane=xk)
        sq_t, sq_k = f512.next()
        st_, sk = sm.next()
        for hh in range(2):
            act(sq_t[:], xt[:, hh * 512:(hh + 1) * 512], AF.Square, [xk], [sq_k, sk], accum=st_[:, hh:hh + 1])
        tt("dve", st_[:, 2:3], st_[:, 0:1], st_[:, 1:2], ALU.add, [sq_k, sk], [sk])
        act(st_[:, 2:3], st_[:, 2:3], AF.Ln, [sk, "vecs"], [sk], scale=1.0 / D, bias=vecs[:, DEPTH * NV_L + 8: DEPTH * NV_L + 9])
        act(st_[:, 3:4], st_[:, 2:3], AF.Exp, [sk], [sk], scale=-0.5)
        ts("dve", xt[:], xt[:], st_[:, 3:4], None, ALU.mult, ALU.bypass, [xk, sk], [xk])
        for g in range(2):
            pt, pk = pmm.next()
            for c4 in range(4):
                c = g * 4 + c4
                tr(pt[:, c4 * 128:(c4 + 1) * 128], xt[:, c * 128:(c + 1) * 128], ident, [xk, "cst"], [pk])
            cp("act", memhT[:, g * 4:(g + 1) * 4, kt * 128:(kt + 1) * 128],
               pt[:].rearrange("p (c t) -> p c t", c=4), [pk], ["memhT"])

    EPSC = vecs[:, DEPTH * NV_L + 8: DEPTH * NV_L + 9]

    def rmsnorm_to_aT(gcol_off, l, halo=False):
        for s in range(NS):
            sl = slice(s * 512, (s + 1) * 512)
            pt, pk = pmm.next()
            for c in range(8):
                bt, bk = b512.next()
                act(bt[:], hT[:, c, sl], AF.Square, [("hT", c, s)], [bk], eng="act")
                mm(pt[:], onesb, bt[:], c == 0, c == 7, [bk, "cbf"], [pk])
            rt, rk = f512.next()
            act(rt[:], pt[:], AF.Ln, [pk, "vecs"], [rk], scale=1.0 / D, bias=EPSC)
            act(rt[:], rt[:], AF.Exp, [rk], [rk], scale=-0.5)
            for c in range(8):
                stt(aT[:, c, 2 + s * 512: 2 + (s + 1) * 512], hT[:, c, sl], vcol(l, gcol_off + c), rt[:],
                    ALU.mult, ALU.mult, [("hT", c, s), rk, "vecs"], [("aT", c, s)])

    def aTs(c, s):
        return aT[:, c, 2 + s * 512: 2 + (s + 1) * 512]

    def aT_reads(s):
        return [("aT", c, s) for c in range(8)]

    def out_proj(w_d2, tag):
        tiles = [wload(wr, [(lambda t: t[:], rows(w_d2)[:, :, hf * 512:(hf + 1) * 512])]) for hf in range(2)]
        for hf in range(2):
            wt, wk = tiles[hf]
            for m4 in range(4):
                m = hf * 4 + m4
                for s in range(NS):
                    pt, pk = pmm.next()
                    for c in range(8):
                        mm(pt[:], wt[:, c, m4 * 128:(m4 + 1) * 128], oT[:, c, s * 512:(s + 1) * 512], c == 0, c == 7,
                           [wk, ("prod", c)], [pk])
                    tt("dve", hT[:, m, s * 512:(s + 1) * 512], hT[:, m, s * 512:(s + 1) * 512], pt[:], ALU.add,
                       [pk, ("hT", m, s)], [("hT", m, s)])

    def head_norm_gate(src, src_key, gate_t, gate_k, l, h, s):
        bt, bk = b512.next()
        act(bt[:], src, AF.Square, [src_key], [bk])
        pt, pk = pmm.next()
        mm(pt[:], onesb, bt[:], True, True, [bk, "cbf"], [pk])
        rt, rk = nrm.next()
        act(rt[:], pt[:], AF.Ln, [pk, "vecs"], [rk], scale=1.0 / 128, bias=EPSC)
        act(rt[:], rt[:], AF.Exp, [rk], [rk], scale=-0.5)
        hh_t, hh_k = nrm.next()
        stt(hh_t[:], src, vcol(l, O_MNG + h), rt[:], ALU.mult, ALU.mult, [src_key, rk, "vecs"], [hh_k])
        tt("dve", oT[:, h, s * 512:(s + 1) * 512], hh_t[:], gate_t[:], ALU.mult, [hh_k, gate_k], [("prod", h)])

    def hgrn2(l):
        j = l // 2
        w_in = hgin_d[j]
        rmsnorm_to_aT(O_GMIX, l)

        def load_head(h):
            return wload(wr, [(lambda t, q=q: t[:, :, q * 128:(q + 1) * 128],
                               rows(w_in)[:, :, q * 1024 + h * 128: q * 1024 + (h + 1) * 128]) for q in range(4)])
        units = [(h, s) for h in range(8) for s in range(NS)]
        wts = {0: load_head(0)}
        U = {}

        def A(i):
            h, s = units[i]
            if s == 0 and h + 1 < 8:
                wts[h + 1] = load_head(h + 1)
            wt, wk = wts[h]
            u = U[i] = dict(h=h, s=s)
            ar = aT_reads(s)
            vt, vk = vtok.next()
            u["vt"], u["vk"] = vt, vk
            for half in range(2):
                pv, pvk = pmm.next()
                for cc in range(4):
                    ch = half * 4 + cc
                    for c in range(8):
                        mm(pv[0:64, cc * 128:(cc + 1) * 128], aT[:, c, 2 + s * 512 + ch * 64: 2 + s * 512 + (ch + 1) * 64],
                           wt[:, c, 256:384], c == 0, c == 7, [wk] + ar, [pvk])
                cp("dve", vt[:, half * 4:(half + 1) * 4, 0:128], pv[0:64, :].rearrange("p (c v) -> p c v", c=4), [pvk], [vk])
            ps3 = []
            for q in (0, 1, 3):
                pt, pk = pmm.next()
                for c in range(8):
                    mm(pt[:], wt[:, c, q * 128:(q + 1) * 128], aTs(c, s), c == 0, c == 7, [wk] + ar, [pk])
                ps3.append((pt, pk))
            u["pq"], u["pz"], u["pg"] = ps3

        def B(i):
            u = U[i]
            h = u["h"]
            (pq, pqk), (pz, pzk), (pg, pgk) = u["pq"], u["pz"], u["pg"]
            qs, qsk = f512.next()
            act(qs[:], pq[:], AF.Silu, [pqk], [qsk])
            gs, gsk = gsr.next()
            act(gs[:], pg[:], AF.Silu, [pgk], [gsk])
            u["gs"], u["gsk"] = gs, gsk
            sg, sgk = f512.next()
            act(sg[:], pz[:], AF.Sigmoid, [pzk], [sgk])
            kk, kkk = f512.next()
            act(kk[:], pz[:], AF.Sigmoid, [pzk], [kkk], scale=-1.0)
            act(sg[:], sg[:], AF.Ln, [sgk, ("omlt", l), ("lbt", l)], [sgk], scale=omlt[:, l, h:h + 1], bias=lbt[:, l, h:h + 1])
            bb, bbk = f512.next()
            P.add("dve", lambda e, bb=bb, sg=sg: e.tensor_tensor_scan(out=bb[:], data0=rmask, data1=sg[:], initial=0.0,
                                                                       op0=ALU.mult, op1=ALU.add), [sgk, "cst"], [bbk])
            e1, e1k = f512.next()
            act(e1[:], bb[:], AF.Exp, [bbk], [e1k])
            act(bb[:], bb[:], AF.Exp, [bbk, e1k], [bbk], scale=-1.0)
            eb, ebk = sm8.next()
            cp("dve", eb[:, 0:8], e1[:].rearrange("p (j t) -> p j t", t=64)[:, :, 63], [e1k], [ebk])
            u["eb"], u["ebk"] = eb, ebk
            qt_, qtk = b512.next()
            tt("dve", qt_[:], qs[:], e1[:], ALU.mult, [qsk, e1k], [qtk])
            kt_, ktk = b512.next()
            stt(kt_[:], kk[:], omlt[:, l, h:h + 1], bb[:], ALU.mult, ALU.mult, [kkk, bbk, ("omlt", l)], [ktk])
            kh, khk = b512.next()
            tt("dve", kh[:].rearrange("p (j t) -> p j t", t=64), kt_[:].rearrange("p (j t) -> p j t", t=64),
               eb[:, 0:8].unsqueeze(2).to_broadcast([128, 8, 64]), ALU.mult, [ktk, ebk], [khk])
            u.update(qt=qt_, qtk=qtk, kt=kt_, ktk=ktk, kh=kh, khk=khk)

        def CDE(i):
            u = U[i]
            trt, trk = psm.next()
            trb = trt[:].bitcast(BF16)
            for ch in range(8):
                tr(trb[0:64, ch * 128:(ch + 1) * 128], u["kh"][:, ch * 64:(ch + 1) * 64], identb, [u["khk"], "cbf"], [trk])
            kT_t, kTk = ktok.next()
            cp("act", kT_t[:], trb[0:64, :].rearrange("p (c v) -> p c v", c=8), [trk], [kTk])
            apt, ak = psm.next()
            for ch in range(8):
                cs = slice(ch * 64, (ch + 1) * 64)
                mm(apt[0:64, cs], u["kt"][:, cs], u["qt"][:, cs], True, True, [u["ktk"], u["qtk"]], [ak])
            am, amk = amr.next()
            tt("dve", am[:].rearrange("p (j t) -> p j t", t=64), apt[0:64, :].rearrange("p (j t) -> p j t", t=64),
               maskT.unsqueeze(1).to_broadcast([64, 8, 64]), ALU.mult, [ak, "cst"], [amk])
            u["am"], u["amk"] = am, amk
            ub = []
            for half in range(2):
                ut, uk = psm.next()
                for cc in range(4):
                    ch = half * 4 + cc
                    mm(ut[:, cc * 128:(cc + 1) * 128], kT_t[:, ch, :], u["vt"][:, ch, 0:128], True, True, [kTk, u["vk"]], [uk])
                ub.append((ut, uk))
            u["ub"] = ub

        def FG(i):
            u = U[i]
            h, s = u["h"], u["s"]
            SK = ("Sst", j, h)
            cp("dve", Sall[:, 0, 0:128], Sst[:, j, h, :], [SK], ["Sall"])
            for ch in range(8):
                ut, uk = u["ub"][ch // 4]
                dst = Sall[:, ch + 1, 0:128] if ch < 7 else Sst[:, j, h, :]
                stt(dst, Sall[:, ch, 0:128], u["eb"][:, ch:ch + 1], ut[:, (ch % 4) * 128:(ch % 4 + 1) * 128], ALU.mult, ALU.add,
                    ["Sall", u["ebk"], uk], ["Sall"] if ch < 7 else [SK])
            sb_, sbk = sallb.next()
            cp("dve", sb_[:, :, :], Sall[:, 0:8, 0:128], ["Sall"], [sbk])
            u["sb"], u["sbk"] = sb_, sbk

        def H(i):
            u = U[i]
            po, pok = pmm.next()
            for ch in range(8):
                cs = slice(ch * 64, (ch + 1) * 64)
                mm(po[:, cs], u["vt"][:, ch, 0:128], u["am"][:, cs], True, False, [u["vk"], u["amk"]], [pok])
                mm(po[:, cs], u["sb"][:, ch, :], u["qt"][:, cs], False, True, [u["sbk"], u["qtk"]], [pok])
            head_norm_gate(po[:], pok, u["gs"], u["gsk"], l, u["h"], u["s"])
            del U[i]

        A(0)
        B(0)
        for i in range(len(units)):
            CDE(i)
            FG(i)
            if i + 1 < len(units):
                A(i + 1)
                B(i + 1)
            H(i)
        out_proj(hgout_d[j], "hgo")

    def mlstm(l):
        j = l // 2
        w_in = mlin_d[j]
        rmsnorm_to_aT(O_GMIX, l)
        B_, U_, MG, M_, WI, CL, LI, LF = (g8[k] for k in ("B", "u", "Mg", "m", "wi", "cl", "li", "lf"))
        gw, gwk = wload(wr, [(lambda t: t[:, :, 0:16], rows(w_in)[:, :, 3072:3088])])
        cp("dve", B_[:, 0:1], mlcar[:, j, 0:1], ["mlcar"], ["g8B"])
        cp("dve", MG[:, 0:1], mlcar[:, j, 1:2], ["mlcar"], ["g8Mg"])
        for s in range(NS):
            ar = aT_reads(s)
            pi, pik = pmm.next()
            pf, pfk = pmm.next()
            for c in range(8):
                mm(pi[0:8, :], gw[:, c, 0:8], aTs(c, s), c == 0, c == 7, [gwk] + ar, [pik])
            for c in range(8):
                mm(pf[0:8, :], gw[:, c, 8:16], aTs(c, s), c == 0, c == 7, [gwk] + ar, [pfk])
            csl = slice(1 + s * 512, 1 + (s + 1) * 512)
            act(LI[:, csl], pi[0:8, :], AF.Identity, [pik, "vecs"], ["g8li"], bias=vecs[0:8, l * NV_L + O_BI: l * NV_L + O_BI + 1])
            act(LF[:, csl], pf[0:8, :], AF.Sigmoid, [pfk, "vecs"], ["g8lf"], bias=vecs[0:8, l * NV_L + O_BF: l * NV_L + O_BF + 1])
            act(LF[:, csl], LF[:, csl], AF.Ln, ["g8lf"], ["g8lf"])
        P.add("dve", lambda e: e.memset(WI[:, :], 1.0), [], ["g8wi"])
        P.add("dve", lambda e: e.tensor_tensor_scan(out=B_[:, 1:T + 1], data0=WI[:, 1:T + 1], data1=LF[:, 1:T + 1],
                                                     initial=B_[:, 0:1], op0=ALU.mult, op1=ALU.add),
              ["g8lf", "g8wi", "g8B"], ["g8B"])
        cp("dve", mlcar[:, j, 0:1], B_[:, T:T + 1], ["g8B"], ["mlcar"])
        tt("dve", U_[:, 1:T + 1], LI[:, 1:T + 1], B_[:, 1:T + 1], ALU.subtract, ["g8li", "g8B"], ["g8li"])
        P.add("dve", lambda e: e.tensor_tensor_scan(out=MG[:, 1:T + 1], data0=U_[:, 1:T + 1], data1=U_[:, 1:T + 1],
                                                     initial=MG[:, 0:1], op0=ALU.max, op1=ALU.max),
              ["g8li", "g8Mg"], ["g8Mg"])
        tt("dve", M_[:, 1:T + 1], B_[:, 1:T + 1], MG[:, 1:T + 1], ALU.add, ["g8B", "g8Mg"], ["g8B"])
        act(CL[:, 1:T + 1], M_[:, 1:T + 1], AF.Exp, ["g8B"], ["g8B"], scale=-1.0)
        mgp = MG[:, 0:T].rearrange("p (j t) -> p j t", t=64)[:, :, 0:1]
        tt("dve", WI[:, 1:T + 1].rearrange("p (j t) -> p j t", t=64), mgp.to_broadcast([8, NCH, 64]),
           MG[:, 1:T + 1].rearrange("p (j t) -> p j t", t=64), ALU.subtract, ["g8Mg", "g8wi"], ["g8wi"])
        act(WI[:, 1:T + 1], WI[:, 1:T + 1], AF.Exp, ["g8wi"], ["g8wi"])
        cp("dve", mlcar[:, j, 1:2], MG[:, T:T + 1], ["g8Mg"], ["mlcar"])
        for g in range(NCH // 8):
            pbc, pbk = psm.next()
            for cc in range(8):
                ch = g * 8 + cc
                tr(pbc[0:64, cc * 8:(cc + 1) * 8], U_[:, 1 + ch * 64: 1 + (ch + 1) * 64], ident[0:8, 0:8], ["g8li", "cst"], [pbk])
            cp("act", uT[:, g * 8:(g + 1) * 8, :], pbc[0:64, 0:64].rearrange("p (c h) -> p c h", c=8), [pbk], ["uT"])

        def load_head(h):
            return wload(wr, [
                (lambda t: t[:, :, 0:64], rows(w_in)[:, :, h * 64:(h + 1) * 64]),
                (lambda t: t[:, :, 64:128], rows(w_in)[:, :, 512 + h * 64: 512 + (h + 1) * 64]),
                (lambda t: t[:, :, 128:256], rows(w_in)[:, :, 1024 + h * 128: 1024 + (h + 1) * 128]),
                (lambda t: t[:, :, 256:384], rows(w_in)[:, :, 2048 + h * 128: 2048 + (h + 1) * 128]),
            ])
        units = [(h, s) for h in range(8) for s in range(NS)]
        wts = {0: load_head(0)}
        U = {}

        def A(i):
            h, s = units[i]
            if s == 0 and h + 1 < 8:
                wts[h + 1] = load_head(h + 1)
            wt, wk = wts[h]
            u = U[i] = dict(h=h, s=s)
            ar = aT_reads(s)
            vt, vk = vtok.next()
            u["vt"], u["vk"] = vt, vk
            for half in range(2):
                pv, pvk = pmm.next()
                for cc in range(4):
                    ch = half * 4 + cc
                    for c in range(8):
                        mm(pv[0:64, cc * 128:(cc + 1) * 128], aT[:, c, 2 + s * 512 + ch * 64: 2 + s * 512 + (ch + 1) * 64],
                           wt[:, c, 128:256], c == 0, c == 7, [wk] + ar, [pvk])
                cp("dve", vt[:, half * 4:(half + 1) * 4, 0:128], pv[0:64, :].rearrange("p (c v) -> p c v", c=4), [pvk], [vk])
            ps3 = []
            for (c0, c1, m) in ((0, 64, 64), (64, 128, 64), (256, 384, 128)):
                pt, pk = pmm.next()
                for c in range(8):
                    mm(pt[0:m, :], wt[:, c, c0:c1], aTs(c, s), c == 0, c == 7, [wk] + ar, [pk])
                ps3.append((pt, pk))
            u["pq"], u["pk"], u["pg"] = ps3
            pkt, pktk = pmm.next()
            for ch in range(8):
                for c in range(8):
                    mm(pkt[0:64, ch * 64:(ch + 1) * 64], aT[:, c, 2 + s * 512 + ch * 64: 2 + s * 512 + (ch + 1) * 64],
                       wt[:, c, 64:128], c == 0, c == 7, [wk] + ar, [pktk])
            u["pkt"], u["pktk"] = pkt, pktk

        def B(i):
            u = U[i]
            h, s = u["h"], u["s"]
            csl = slice(1 + s * 512, 1 + (s + 1) * 512)
            (pq, pqk), (pk_, pkk), (pg, pgk) = u["pq"], u["pk"], u["pg"]
            gs, gsk = gsr.next()
            act(gs[:], pg[:], AF.Sigmoid, [pgk], [gsk])
            u["gs"], u["gsk"] = gs, gsk
            kTs, kTk = b512.next()
            act(kTs[0:64, :], pk_[0:64, :], AF.Copy, [pkk], [kTk], scale=0.125)
            qs, qsk = b512.next()
            cp("act", qs[0:64, :], pq[0:64, :], [pqk], [qsk])
            selh = sel[:, h * 128:(h + 1) * 128]
            pbc, pbk = psm.next()
            mm(pbc[:], selh, MG[:, csl], True, True, ["g8Mg", "cst"], [pbk])
            mgp_t, mgpk = sm8.next()
            cp("act", mgp_t[0:64, 0:8], pbc[0:64, :].rearrange("p (j t) -> p j t", t=64)[:, :, 63], [pbk], [mgpk])
            mgb, mgbk = f512.next()
            tt("dve", mgb[0:64, :].rearrange("p (j t) -> p j t", t=64), negmT.unsqueeze(1).to_broadcast([64, 8, 64]),
               pbc[0:64, :].rearrange("p (j t) -> p j t", t=64), ALU.subtract, [pbk, "cst"], [mgbk])
            uh = uT[:, s * 8:(s + 1) * 8, h]
            tt("dve", mgb[0:64, :].rearrange("p (j t) -> p j t", t=64), mgb[0:64, :].rearrange("p (j t) -> p j t", t=64),
               uh.unsqueeze(2).to_broadcast([64, 8, 64]), ALU.add, [mgbk, "uT"], [mgbk])
            act(mgb[0:64, :], mgb[0:64, :], AF.Exp, [mgbk], [mgbk])
            u["dm"], u["dmk"] = mgb, mgbk
            w_t, w_k = sm8.next()
            tt("dve", w_t[0:64, 0:8], uh, mgp_t[0:64, 0:8], ALU.subtract, ["uT", mgpk], [w_k])
            act(w_t[0:64, 0:8], w_t[0:64, 0:8], AF.Exp, [w_k], [w_k])
            kw, kwk = ktok.next()
            stt(kw[:, :, 0:64], u["pkt"][0:64, :].rearrange("p (j d) -> p j d", d=64), 0.125,
                w_t[0:64, 0:8].unsqueeze(2).to_broadcast([64, 8, 64]), ALU.mult, ALU.mult, [u["pktk"], w_k], [kwk])
            u["kw"], u["kwk"] = kw, kwk
            pbc, pbk = psm.next()
            mm(pbc[:], selh, WI[:, csl], True, True, ["g8wi", "cst"], [pbk])
            dec, deck = sm8.next()
            cp("act", dec[0:64, 0:8], pbc[0:64, :].rearrange("p (j t) -> p j t", t=64)[:, :, 63], [pbk], [deck])
            u["dec"], u["deck"] = dec, deck
            qw, qwk = b512.next()
            tt("dve", qw[0:64, :], pbc[0:64, :], qs[0:64, :], ALU.mult, [qsk, pbk], [qwk])
            pbc, pbk = psm.next()
            mm(pbc[:], selh, CL[:, csl], True, True, ["g8B", "cst"], [pbk])
            clb, clbk = clr.next()
            cp("act", clb[:], pbc[:], [pbk], [clbk])
            u.update(kTs=kTs, kTk=kTk, qs=qs, qsk=qsk, qw=qw, qwk=qwk, clb=clb, clbk=clbk)

        def CDE(i):
            u = U[i]
            stp, stk = psm.next()
            for ch in range(8):
                cs = slice(ch * 64, (ch + 1) * 64)
                mm(stp[0:64, cs], u["kTs"][0:64, cs], u["qs"][0:64, cs], True, True, [u["kTk"], u["qsk"]], [stk])
            sT, sTk = amr.next()
            tt("dve", sT[:], stp[0:64, :], u["dm"][0:64, :], ALU.mult, [stk, u["dmk"]], [sTk])
            u["sT"], u["sTk"] = sT, sTk
            cb = []
            for g3 in range(3):
                ct, ck = psm.next()
                for cc in range(3):
                    ch = g3 * 3 + cc
                    if ch < 8:
                        mm(ct[0:64, cc * 160: cc * 160 + 129], u["kw"][:, ch, 0:64], u["vt"][:, ch, 0:129], True, True,
                           [u["kwk"], u["vk"]], [ck])
                cb.append((ct, ck))
            u["cb"] = cb

        def FG(i):
            u = U[i]
            h = u["h"]
            CK = ("Cst", j, h)
            cp("dve", Sall[0:64, 0, :], Cst[:, j, h, :], [CK], ["Sall"])
            for ch in range(8):
                ct, ck = u["cb"][ch // 3]
                dst = Sall[0:64, ch + 1, :] if ch < 7 else Cst[:, j, h, :]
                stt(dst, Sall[0:64, ch, :], u["dec"][0:64, ch:ch + 1], ct[0:64, (ch % 3) * 160:(ch % 3) * 160 + 129], ALU.mult, ALU.add,
                    ["Sall", u["deck"], ck], ["Sall"] if ch < 7 else [CK])
            sb_, sbk = sallb.next()
            cp("dve", sb_[0:64, :, :], Sall[0:64, 0:8, 0:128], ["Sall"], [sbk])
            nb, nbk = sallb.next()
            tt("dve", nb[0:64, :, :], onesb[0:64, :].unsqueeze(1).to_broadcast([64, 8, 128]),
               Sall[0:64, 0:8, 128:129].to_broadcast([64, 8, 128]), ALU.mult, ["Sall", "cbf"], [nbk])
            u.update(sb=sb_, sbk=sbk, nb=nb, nbk=nbk)

        def H(i):
            u = U[i]
            pnum, pnk = pmm.next()
            pden, pdk = pmm.next()
            for ch in range(8):
                cs = slice(ch * 64, (ch + 1) * 64)
                mm(pnum[:, cs], u["vt"][:, ch, 0:128], u["sT"][:, cs], True, False, [u["vk"], u["sTk"]], [pnk])
                mm(pnum[:, cs], u["sb"][0:64, ch, :], u["qw"][0:64, cs], False, True, [u["sbk"], u["qwk"]], [pnk])
            for ch in range(8):
                cs = slice(ch * 64, (ch + 1) * 64)
                mm(pden[:, cs], onesb[0:64, :], u["sT"][:, cs], True, False, ["cbf", u["sTk"]], [pdk])
                mm(pden[:, cs], u["nb"][0:64, ch, :], u["qw"][0:64, cs], False, True, [u["nbk"], u["qwk"]], [pdk])
            dd, ddk = f512.next()
            act(dd[:], pden[:], AF.Abs, [pdk], [ddk])
            tt("dve", dd[:], dd[:], u["clb"][:], ALU.max, [ddk, u["clbk"]], [ddk])
            act(dd[:], dd[:], AF.Ln, [ddk], [ddk])
            act(dd[:], dd[:], AF.Exp, [ddk], [ddk], scale=-1.0)
            hh, hhk = f512.next()
            tt("dve", hh[:], pnum[:], dd[:], ALU.mult, [pnk, ddk], [hhk])
            head_norm_gate(hh[:], hhk, u["gs"], u["gsk"], l, u["h"], u["s"])
            del U[i]

        A(0)
        B(0)
        for i in range(len(units)):
            CDE(i)
            FG(i)
            if i + 1 < len(units):
                A(i + 1)
                B(i + 1)
            H(i)
        out_proj(mlout_d[j], "mlo")

    def xattn(l):
        rmsnorm_to_aT(O_GXA, l)
        for c in range(8):
            ts("dve", memnT[:, c, :], memhT[:, c, :], vcol(l, O_GMEM + c), None, ALU.mult, ALU.bypass,
               ["memhT", "vecs"], ["memnT"])
        for hf in range(2):
            wt, wk = wload(wr, [(lambda t: t[:], rows(wkv_d[l])[:, :, hf * 512:(hf + 1) * 512])])
            for m4 in range(4):
                pt, pk = pmm.next()
                for c in range(8):
                    mm(pt[:, 0:NMEM], wt[:, c, m4 * 128:(m4 + 1) * 128], memnT[:, c, :], c == 0, c == 7, [wk, "memnT"], [pk])
                cp("act", KT[:, hf * 4 + m4, :], pt[:, 0:NMEM], [pk], ["KT"])
        for hf in range(2):
            wt, wk = wload(wr, [(lambda t: t[:], rows(wkv_d[l])[:, :, D + hf * 512: D + (hf + 1) * 512])])
            for kt in range(2):
                pt, pk = pmm.next()
                for c in range(8):
                    mm(pt[:], memnT[:, c, kt * 128:(kt + 1) * 128], wt[:, c, :], c == 0, c == 7, [wk, "memnT"], [pk])
                cp("act", Vt[:, kt, hf * 512:(hf + 1) * 512], pt[:], [pk], ["Vt"])
        for hp in range(2):
            wt, wk = wload(wr, [(lambda t: t[:], rows(wq_d[l])[:, :, hp * 512:(hp + 1) * 512])])
            for hh_ in range(2):
                hd = hp * 2 + hh_
                for s in range(NS):
                    ar = aT_reads(s)
                    q0, q0k = qx.next()
                    q1, q1k = qx.next()
                    qts = ((q0, q0k), (q1, q1k))
                    for dc in range(2):
                        pt, pk = pmm.next()
                        for c in range(8):
                            mm(pt[:], wt[:, c, hh_ * 256 + dc * 128: hh_ * 256 + (dc + 1) * 128], aTs(c, s), c == 0, c == 7,
                               [wk] + ar, [pk])
                        act(qts[dc][0][:], pt[:], AF.Copy, [pk], [qts[dc][1]], scale=1.0 / 16.0)
                    po0, po0k = pmm.next()
                    po1, po1k = pmm.next()
                    pos = ((po0, po0k), (po1, po1k))
                    for tt_ in range(4):
                        tsl = slice(tt_ * 128, (tt_ + 1) * 128)
                        sct, sck = psm.next()
                        sc = sct[:, 0:256]
                        scks = [sck]
                        for dc in range(2):
                            mm(sc, qts[dc][0][:, tsl], KT[:, hd * 2 + dc, :], dc == 0, dc == 1, [qts[dc][1], "KT"], scks)
                        st_, sk = sm.next()
                        P.add("dve", lambda e, st_=st_, sc=sc: e.tensor_reduce(out=st_[:, 0:1], in_=sc, axis=AX.X, op=ALU.max, negate=True),
                              scks, [sk])
                        ee, eek = f512.next()
                        act(ee[:, 0:256], sc, AF.Exp, scks + [sk], [eek, sk], bias=st_[:, 0:1], accum=st_[:, 1:2])
                        recip(st_[:, 2:3], st_[:, 1:2], [eek, sk], [sk])
                        pp, ppk = b512.next()
                        ts("dve", pp[:, 0:256], ee[:, 0:256], st_[:, 2:3], None, ALU.mult, ALU.bypass, [eek, sk], [ppk])
                        trt, trk = psm.next()
                        trb = trt[:].bitcast(BF16)
                        for kt in range(2):
                            tr(trb[:, kt * 128:(kt + 1) * 128], pp[:, kt * 128:(kt + 1) * 128], identb, [ppk, "cbf"], [trk])
                        pT, pTk = b512.next()
                        cp("act", pT[:, 0:256], trb[:, 0:256], [trk], [pTk])
                        for dc in range(2):
                            for kt in range(2):
                                mm(pos[dc][0][:, tsl], Vt[:, kt, hd * 256 + dc * 128: hd * 256 + (dc + 1) * 128],
                                   pT[:, kt * 128:(kt + 1) * 128], kt == 0, kt == 1, ["Vt", pTk], [pos[dc][1]])
                    for dc in range(2):
                        cp("act", oT[:, hd * 2 + dc, s * 512:(s + 1) * 512], pos[dc][0][:], [pos[dc][1]], [("prod", hd * 2 + dc)])
        out_proj(wo_d[l], "xo")

    assert T == 512
    nsub = 2
    nn = 256
    bnds = [0, 256, 512]

    def ffn(l, wave):
        rmsnorm_to_aT(O_GFFN, l)
        cp("dve", aT[:, :, 0:2], ffhalo[:, l, :, :], ["ffhalo"], [("aTh",)])
        cp("dve", ffhalo[:, l, :, :], aT[:, :, T:T + 2], [("aT", c, NS - 1) for c in range(8)], ["ffhalo"])

        def load_g(g):
            return wload(wr, [(lambda t: t[:, :, 0:256], rows(wup_d[l])[:, :, g * 256:(g + 1) * 256]),
                              (lambda t: t[:, :, 256:512], rows(wup_d[l])[:, :, DFF + g * 256: DFF + (g + 1) * 256])])
        nxt = load_g(0)
        for g in range(NFF // 2):
            wt, wk = nxt
            if g + 1 < NFF // 2:
                nxt = load_g(g + 1)
            for jj in range(2):
                jf = g * 2 + jj
                rd = [wk, ("aTh",)] + [("aT", c, s) for c in range(8) for s in range(NS)]
                G, gk = pdb_next()
                V, vk_ = pdb_next()
                for si in range(nsub):
                    a, b = bnds[si], bnds[si + 1]
                    for c in range(8):
                        mm(G[:, si * 512: si * 512 + nn + 2], wt[:, c, jj * 128:(jj + 1) * 128], aT[:, c, a:b + 2], c == 0, c == 7, rd, gk)
                    for c in range(8):
                        mm(V[:, si * 512: si * 512 + nn + 2], wt[:, c, 256 + jj * 128: 256 + (jj + 1) * 128], aT[:, c, a:b + 2], c == 0, c == 7,
                           rd, vk_)
                Gv = G[:].rearrange("p (s n) -> p s n", s=2)
                Vv = V[:].rearrange("p (s n) -> p s n", s=2)
                halves = []
                for (Pv, pk_, col) in ((Gv, gk, jf), (Vv, vk_, NFF + jf)):
                    u_, uk_ = fu.next()
                    y, yk = f512.next()
                    u3 = u_[:, 0:2 * (nn + 2)].rearrange("p (s n) -> p s n", s=2)
                    y3 = y[:, 0:2 * nn].rearrange("p (s n) -> p s n", s=2)
                    act(u3[:, :, 0:nn + 1], Pv[:, :, 0:nn + 1], AF.Copy, pk_, [uk_])
                    act(y3, Pv[:, :, 2:nn + 2], AF.Identity, pk_ + ["vecs"], [yk], scale=vcol(l, O_CW2 + col), bias=vcol(l, O_CB + col))
                    halves.append((u3, uk_, y3, yk, col, y))
                for (u3, uk_, y3, yk, col, y) in halves:
                    stt(y3, u3[:, :, 1:nn + 1], vcol(l, O_CW1 + col), y3, ALU.mult, ALU.add, [uk_, yk, "vecs"], [yk])
                for (u3, uk_, y3, yk, col, y) in halves:
                    stt(y3, u3[:, :, 0:nn], vcol(l, O_CW0 + col), y3, ALU.mult, ALU.add, [uk_, yk, "vecs"], [yk])
                (_, _, _, ygk, _, yg), (_, _, _, yvk, _, yv) = halves
                sgb, sgbk = b512.next()
                act(sgb[:, 0:T], yg[:, 0:T], AF.Silu, [ygk], [sgbk])
                tt("dve", prod[:, jf, :], sgb[:, 0:T], yv[:, 0:T], ALU.mult, [sgbk, yvk], [("prod", jf)])
        def load_d(mp):
            return wload(wdr, [(lambda t: t[:], wdn_d[l].rearrange("(j p) n -> p j n", p=128)[:, :, mp * 128:(mp + 1) * 128])])
        nxt = load_d(0)
        for mp in range(8):
            wt, wk = nxt
            if mp + 1 < 8:
                nxt = load_d(mp + 1)
            for m2 in range(1):
                m = mp
                for s in range(NS):
                    pt, pk = pmm.next()
                    for jf in range(NFF):
                        mm(pt[:], wt[:, jf, m2 * 128:(m2 + 1) * 128], prod[:, jf, s * 512:(s + 1) * 512], jf == 0, jf == NFF - 1,
                           [wk, ("prod", jf)], [pk])
                    tt("dve", hT[:, m, s * 512:(s + 1) * 512], hT[:, m, s * 512:(s + 1) * 512], pt[:], ALU.add,
                       [pk, ("hT", m, s)], [("hT", m, s)])

    olanes = []
    for w in range(NW):
        for t4 in range(T // 128):
            s = t4 // 4
            xt, xk = xin.next()
            r0 = w * T + t4 * 128
            P.dma("sp", xt[:], x_d[r0:r0 + 128, :], writes=[xk], lane=xk)
            for g in range(2):
                pt, pk = pmm.next()
                for c4 in range(4):
                    c = g * 4 + c4
                    tr(pt[:, c4 * 128:(c4 + 1) * 128], xt[:, c * 128:(c + 1) * 128], ident, [xk, "cst"], [pk])
                cp("act", hT[:, g * 4:(g + 1) * 4, t4 * 128:(t4 + 1) * 128], pt[:].rearrange("p (c t) -> p c t", c=4), [pk],
                   [("hT", c, s) for c in range(g * 4, g * 4 + 4)])
        for l in layers:
            if "m" in phases:
                if l % 2 == 0:
                    hgrn2(l)
                else:
                    mlstm(l)
            if "x" in phases:
                xattn(l)
            if "f" in phases:
                ffn(l, w)
        for s in range(NS):
            sl = slice(s * 512, (s + 1) * 512)
            if final:
                pt, pk = pmm.next()
                for c in range(8):
                    bt, bk = b512.next()
                    act(bt[:], hT[:, c, sl], AF.Square, [("hT", c, s)], [bk])
                    mm(pt[:], onesb, bt[:], c == 0, c == 7, [bk, "cbf"], [pk])
                rt, rk = nrm.next()
                act(rt[:], pt[:], AF.Ln, [pk, "vecs"], [rk], scale=1.0 / D, bias=EPSC)
                act(rt[:], rt[:], AF.Exp, [rk], [rk], scale=-0.5)
            for t4 in range(4):
                tsl = slice(s * 512 + t4 * 128, s * 512 + (t4 + 1) * 128)
                xt, xk = xin.next()
                for g in range(2):
                    pt2, pk2 = pmm.next()
                    for c4 in range(4):
                        c = g * 4 + c4
                        if final:
                            yt, yk = f512.next()
                            stt(yt[:, 0:128], hT[:, c, tsl], vecs[:, DEPTH * NV_L + c: DEPTH * NV_L + c + 1], rt[:, t4 * 128:(t4 + 1) * 128], ALU.mult, ALU.mult,
                                [("hT", c, s), rk, "vecs"], [yk])
                            tr(pt2[:, c4 * 128:(c4 + 1) * 128], yt[:, 0:128], ident, [yk, "cst"], [pk2])
                        else:
                            tr(pt2[:, c4 * 128:(c4 + 1) * 128], hT[:, c, tsl], ident, [("hT", c, s), "cst"], [pk2])
                    cp("act", xt[:, g * 512:(g + 1) * 512], pt2[:], [pk2], [xk])
                r0 = w * T + s * 512 + t4 * 128
                ln = ("o", xk)
                P.dma("sp", out_d[r0:r0 + 128, :], xt[:], reads=[xk], writes=[("outrow", r0)], lane=ln)
                if ln not in olanes:
                    olanes.append(ln)
    P.finish(final_lanes=olanes)
    return nc, P.stats


def _consts():
    cst = np.zeros((128, 128 + 64 + 64 + 512 + 1024), np.float32)
    cst[:, 0:128] = np.eye(128, dtype=np.float32)
    s = np.arange(64)[:, None]
    t = np.arange(64)[None, :]
    cst[0:64, 128:192] = (s <= t).astype(np.float32)
    cst[0:64, 192:256] = np.where(s <= t, 0.0, -1e30).astype(np.float32)
    rm = np.ones(512, np.float32)
    rm[::64] = 0.0
    cst[:, 256:768] = rm[None, :]
    for h in range(8):
        cst[h, 768 + h * 128: 768 + (h + 1) * 128] = 1.0
    cbf = np.zeros((128, 256), np.float32)
    cbf[:, 0:128] = np.eye(128, dtype=np.float32)
    cbf[:, 128:256] = 1.0
    return cst, cbf.astype(ml_dtypes.bfloat16)


def _cols(v, n):
    return np.ascontiguousarray(np.asarray(v, np.float32).reshape(n, 128).T)


def _pack_vecs(inp):
    vec = np.zeros((128, DEPTH * NV_L + 16), np.float32)
    for l in range(DEPTH):
        b = l * NV_L
        j = l // 2
        vec[:, b + 0:b + 8] = _cols(inp["norm_mix_g"][l], 8)
        vec[:, b + 8:b + 16] = _cols(inp["norm_xa_g"][l], 8)
        vec[:, b + 16:b + 24] = _cols(inp["norm_mem_g"][l], 8)
        vec[:, b + 24:b + 32] = _cols(inp["norm_ffn_g"][l], 8)
        vec[:, b + 32:b + 76] = _cols(inp["ffn_conv_w"][l][0], 44)
        vec[:, b + 76:b + 120] = _cols(inp["ffn_conv_w"][l][1], 44)
        vec[:, b + 120:b + 164] = _cols(inp["ffn_conv_w"][l][2], 44)
        vec[:, b + 164:b + 208] = _cols(inp["ffn_conv_b"][l], 44)
        vec[:, b + 208:b + 216] = _cols((inp["hg_norm_g"] if l % 2 == 0 else inp["ml_norm_g"])[j], 8)
        vec[:, b + 216:b + 224] = _cols(inp["hg_lb_logits"][l], 8)
        if l % 2 == 1:
            vec[0:8, b + 224] = np.asarray(inp["ml_b_gate"][j][0:8], np.float32)
            vec[0:8, b + 225] = np.asarray(inp["ml_b_gate"][j][8:16], np.float32)
    vec[:, DEPTH * NV_L: DEPTH * NV_L + 8] = _cols(inp["final_g"], 8)
    vec[:, DEPTH * NV_L + 8] = EPS
    return vec


_CACHE = {}
MODE = "fused"
WAVE_T = 512

_WNAMES = ("hg_w_in", "hg_w_out", "ml_w_in", "ml_w_out", "xa_wq", "xa_wkv", "xa_wo", "ffn_w_up", "ffn_w_down")


def _get_nc(**kw):
    key = tuple(sorted(kw.items()))
    if key not in _CACHE:
        _CACHE[key] = build(**kw)[0]
    return _CACHE[key]


def _launch(nc, xs, inp, vec, cst, cbf, n):
    shared = {k: np.ascontiguousarray(inp[k], dtype=np.float32) for k in _WNAMES}
    in_maps = []
    for b in range(n):
        m = dict(shared)
        m["x"] = np.ascontiguousarray(xs[b], dtype=np.float32)
        m["mem"] = np.ascontiguousarray(inp["mem"][b], dtype=np.float32)
        m["vecs"] = vec
        m["cst"] = cst
        m["cbf"] = cbf
        in_maps.append(m)
    res = run_bass_kernel_spmd(nc, in_maps, core_ids=list(range(n)))
    return [r["out"] for r in res.results]


def kernel(**inputs):
    inp = {k: np.asarray(v) for k, v in inputs.items()}
    vec = _pack_vecs(inp)
    cst, cbf = _consts()
    x = inp["x"]
    n = x.shape[0]
    S = x.shape[1]
    if MODE == "fused":
        nc = _get_nc(S=S, T=WAVE_T, layers=(0, 1, 2, 3), final=True)
        outs = _launch(nc, [x[b] for b in range(n)], inp, vec, cst, cbf, n)
    else:
        hs = [x[b] for b in range(n)]
        for l in range(DEPTH):
            nc = _get_nc(S=S, T=WAVE_T, layers=(l,), final=(l == DEPTH - 1))
            hs = _launch(nc, hs, inp, vec, cst, cbf, n)
        outs = hs
    return np.stack(outs, axis=0).astype(np.float32)
```
